# Optimizing a Trainium2 kernel written in Bass

```python
import math
import jax, jax.numpy as jnp
from jax import lax
import numpy as np

D_MODEL = 1024
BATCH = 8
SEQ = 2048
DEPTH = 2

HEAD_DIM = 64
SB_HEADS = 8
SB_WIDTH = SB_HEADS * HEAD_DIM
CONV_CH = 512
CONV_WIDTH = 3
ATT_HEADS = 16
KV_HEADS = 4
GQA_REP = ATT_HEADS // KV_HEADS
ATT_WIDTH = ATT_HEADS * HEAD_DIM
KV_WIDTH = KV_HEADS * HEAD_DIM
IDX_HEADS = 8
IDX_DIM = 64
TOPK_MAX = 256
REL_BUCKETS = 32
REL_MAX_DIST = 128
FFN_HIDDEN = ((-(-8 * D_MODEL // 3) + 255) // 256) * 256
QBLOCK = 128
EPS = 1e-6
N_EVEN = (DEPTH + 1) // 2
N_ODD = DEPTH // 2
EVEN_IN = 3 * SB_WIDTH + 3 * CONV_CH
ODD_IN = ATT_WIDTH + 2 * KV_WIDTH + IDX_HEADS * IDX_DIM + IDX_DIM + IDX_HEADS

kernel_name = "hybrid_stickbreak_shortconv_dsa_block"


def rms_norm(x, g):
    xf = x.astype(jnp.float32)
    y = xf * lax.rsqrt(jnp.mean(xf * xf, axis=-1, keepdims=True) + EPS)
    return (y * g.astype(jnp.float32)).astype(x.dtype)


def offsets(widths):
    out, acc = [], 0
    for w in widths[:-1]:
        acc += w
        out.append(acc)
    return out


def to_blocks(a):
    b, s = a.shape[:2]
    a = a.reshape((b, s // QBLOCK, QBLOCK) + a.shape[2:])
    return jnp.moveaxis(a, 1, 0)


def from_blocks(a):
    a = jnp.moveaxis(a, 0, 1)
    return a.reshape((a.shape[0], a.shape[1] * a.shape[2]) + a.shape[3:])


def stick_breaking_attention(q, k, v):
    s = q.shape[1]
    scale = HEAD_DIM ** -0.5
    kpos = jnp.arange(s)

    def block(args):
        qb, qpos = args
        z = jnp.einsum('bqhd,bkhd->bhqk', qb, k).astype(jnp.float32) * scale
        mask = kpos[None, :] < qpos[:, None]
        log_beta = jax.nn.log_sigmoid(z)
        log_1m_beta = jnp.where(mask, log_beta - z, 0.0)
        after = lax.cumsum(log_1m_beta, axis=3, reverse=True) - log_1m_beta
        w = jnp.where(mask, jnp.exp(log_beta + after), 0.0)
        return jnp.einsum('bhqk,bkhd->bqhd', w.astype(v.dtype), v)

    qpos = jnp.arange(s).reshape(-1, QBLOCK)
    return from_blocks(lax.map(block, (to_blocks(q), qpos)))


def short_conv_mixer(b_gate, c_gate, u, conv_w):
    g = c_gate * u
    y = lax.conv_general_dilated(
        g, conv_w[:, None, :], window_strides=(1,),
        padding=[(CONV_WIDTH - 1, 0)],
        dimension_numbers=('NWC', 'WIO', 'NWC'),
        feature_group_count=CONV_CH)
    return b_gate * y


def rel_bucket(dist):
    exact = REL_BUCKETS // 2
    d_f = jnp.maximum(dist, 1).astype(jnp.float32)
    large = exact + (jnp.log(d_f / exact) / math.log(REL_MAX_DIST / exact)
                     * (REL_BUCKETS - exact)).astype(jnp.int32)
    large = jnp.minimum(large, REL_BUCKETS - 1)
    return jnp.where(dist < exact, dist, large)


def dsa_attention(q, k, v, q_idx, k_idx, w_idx, rel_bias):
    s = q.shape[1]
    topk = min(TOPK_MAX, s // 4)
    kpos = jnp.arange(s)
    gather = jax.vmap(lambda a, i: a[i])

    def block(args):
        qb, qib, wb, qpos = args
        dots = jnp.einsum('bqhd,bkd->bqhk', qib, k_idx).astype(jnp.float32) * IDX_DIM ** -0.5
        score = jnp.einsum('bqh,bqhk->bqk', wb.astype(jnp.float32) * IDX_HEADS ** -0.5,
                           jax.nn.relu(dots))
        causal = kpos[None, :] <= qpos[:, None]
        score = jnp.where(causal[None], score, -jnp.inf)
        _, idx = lax.top_k(score, topk)
        valid = idx <= qpos[None, :, None]
        k_sel = gather(k, idx)
        v_sel = gather(v, idx)
        bucket = rel_bucket(jnp.maximum(qpos[None, :, None] - idx, 0))
        bias = rel_bias[bucket].reshape(idx.shape + (KV_HEADS, GQA_REP))
        bias = jnp.moveaxis(bias, 2, -1)
        logits = (jnp.einsum('bqgrd,bqkgd->bqgrk', qb, k_sel).astype(jnp.float32)
                  * HEAD_DIM ** -0.5 + bias.astype(jnp.float32))
        logits = jnp.where(valid[:, :, None, None, :], logits, -jnp.inf)
        p = jax.nn.softmax(logits, axis=-1)
        return jnp.einsum('bqgrk,bqkgd->bqgrd', p.astype(v.dtype), v_sel)

    qpos = jnp.arange(s).reshape(-1, QBLOCK)
    out = lax.map(block, (to_blocks(q), to_blocks(q_idx), to_blocks(w_idx), qpos))
    return from_blocks(out)


def even_mixer(h, w_in, conv_w, w_out):
    b, s, _ = h.shape
    proj = h @ w_in
    q, k, v, bg, cg, u = jnp.split(
        proj, offsets([SB_WIDTH, SB_WIDTH, SB_WIDTH, CONV_CH, CONV_CH, CONV_CH]), axis=-1)
    heads = lambda a: a.reshape(b, s, SB_HEADS, HEAD_DIM)
    a_out = stick_breaking_attention(heads(q), heads(k), heads(v)).reshape(b, s, SB_WIDTH)
    b_out = short_conv_mixer(bg, cg, u, conv_w)
    return jnp.concatenate([a_out, b_out], axis=-1) @ w_out


def odd_mixer(h, w_in, q_gain, k_gain, w_out, rel_bias):
    b, s, _ = h.shape
    proj = h @ w_in
    q, k, v, qi, ki, wi = jnp.split(
        proj, offsets([ATT_WIDTH, KV_WIDTH, KV_WIDTH, IDX_HEADS * IDX_DIM, IDX_DIM, IDX_HEADS]),
        axis=-1)
    q = rms_norm(q.reshape(b, s, ATT_HEADS, HEAD_DIM), q_gain)
    q = q.reshape(b, s, KV_HEADS, GQA_REP, HEAD_DIM)
    k = rms_norm(k.reshape(b, s, KV_HEADS, HEAD_DIM), k_gain)
    v = v.reshape(b, s, KV_HEADS, HEAD_DIM)
    qi = qi.reshape(b, s, IDX_HEADS, IDX_DIM)
    out = dsa_attention(q, k, v, qi, ki, wi, rel_bias).reshape(b, s, ATT_WIDTH)
    return out @ w_out


def swiglu(h, w_gate, w_up, w_down):
    return (jax.nn.silu(h @ w_gate) * (h @ w_up)) @ w_down


def setup_inputs(seed: int = 0) -> dict:
    key = jax.random.key(seed)
    ks = jax.random.split(key, 14)
    f32 = jnp.float32
    nrm = lambda k, shape, fan_in: jax.random.normal(k, shape, f32) * fan_in ** -0.5
    return {
        'x': jax.random.normal(ks[0], (BATCH, SEQ, D_MODEL), f32),
        'norm_mix': 1.0 + 0.01 * jax.random.normal(ks[1], (DEPTH, D_MODEL), f32),
        'norm_ffn': 1.0 + 0.01 * jax.random.normal(ks[2], (DEPTH, D_MODEL), f32),
        'ev_w_in': nrm(ks[3], (N_EVEN, D_MODEL, EVEN_IN), D_MODEL),
        'ev_conv_w': nrm(ks[4], (N_EVEN, CONV_WIDTH, CONV_CH), CONV_WIDTH),
        'ev_w_out': nrm(ks[5], (N_EVEN, SB_WIDTH + CONV_CH, D_MODEL), SB_WIDTH + CONV_CH),
        'od_w_in': nrm(ks[6], (N_ODD, D_MODEL, ODD_IN), D_MODEL),
        'od_q_gain': 1.0 + 0.01 * jax.random.normal(ks[7], (N_ODD, HEAD_DIM), f32),
        'od_k_gain': 1.0 + 0.01 * jax.random.normal(ks[8], (N_ODD, HEAD_DIM), f32),
        'od_w_out': nrm(ks[9], (N_ODD, ATT_WIDTH, D_MODEL), ATT_WIDTH),
        'rel_bias': 0.5 * jax.random.normal(ks[10], (REL_BUCKETS, ATT_HEADS), f32),
        'ffn_w_gate': nrm(ks[11], (DEPTH, D_MODEL, FFN_HIDDEN), D_MODEL),
        'ffn_w_up': nrm(ks[12], (DEPTH, D_MODEL, FFN_HIDDEN), D_MODEL),
        'ffn_w_down': nrm(ks[13], (DEPTH, FFN_HIDDEN, D_MODEL), FFN_HIDDEN),
    }


def reference(x, norm_mix, norm_ffn, ev_w_in, ev_conv_w, ev_w_out, od_w_in, od_q_gain,
              od_k_gain, od_w_out, rel_bias, ffn_w_gate, ffn_w_up, ffn_w_down):
    for layer in range(DEPTH):
        i = layer // 2
        h = rms_norm(x, norm_mix[layer])
        if layer % 2 == 0:
            x = x + even_mixer(h, ev_w_in[i], ev_conv_w[i], ev_w_out[i])
        else:
            x = x + odd_mixer(h, od_w_in[i], od_q_gain[i], od_k_gain[i], od_w_out[i], rel_bias)
        h = rms_norm(x, norm_ffn[layer])
        x = x + swiglu(h, ffn_w_gate[layer], ffn_w_up[layer], ffn_w_down[layer])
    return x
```

```python
import math
from contextlib import ExitStack

import numpy as np
import ml_dtypes
import concourse.bass as bass
import concourse.mybir as mybir
from concourse.bass_utils import run_bass_kernel_spmd

ACT = mybir.ActivationFunctionType
ALU = mybir.AluOpType
F32 = mybir.dt.float32
BF16 = mybir.dt.bfloat16
AX = mybir.AxisListType

S_LEN = 2048
D = 1024
FF = 2816
NFC = FF // 128
EPS = 1e-6
BIG = 32768.0
TOPK = 256
NBIS = 16
USE_FAST_RECIP = False
ENGS = ("pe", "act", "dve", "pool", "sp")


class Op:
    __slots__ = ("eng", "fn", "deps", "idx", "sig", "dma_key", "needed")

    def __init__(self, eng, fn):
        self.eng = eng
        self.fn = fn
        self.deps = []
        self.idx = -1
        self.sig = None
        self.dma_key = None
        self.needed = False


class T:
    __slots__ = ("name", "writer", "readers", "pending")

    def __init__(self, name, pending=()):
        self.name = name
        self.writer = None
        self.readers = []
        self.pending = list(pending)


def _compress(ops):
    last = {}
    out = []
    for o in ops:
        if o.dma_key is not None:
            out.append(o)
        elif o.eng not in last or last[o.eng].idx < o.idx:
            last[o.eng] = o
    out.extend(last.values())
    return out


class Region:
    def __init__(self, arena, name, off, size, pending):
        self.arena = arena
        self.name = name
        self.off = off
        self.size = size
        self.pending = pending
        self.tiles = []

    def f32(self):
        return self.arena.ap[:, self.off:self.off + self.size]

    def bf16(self):
        return self.arena.ap[:, self.off:self.off + self.size].bitcast(BF16)

    def tile(self, name=None):
        t = T(name or self.name, self.pending)
        self.tiles.append(t)
        return t

    def collect(self):
        ops = list(self.pending)
        for t in self.tiles:
            if t.writer is not None:
                ops.append(t.writer)
            ops.extend(t.readers)
        return _compress(ops)

    def retile(self, name=None):
        self.pending = self.collect()
        self.tiles = []
        return self.tile(name)


class Arena:
    def __init__(self, ap, words):
        self.ap = ap
        self.words = words
        self.live = []
        self.freed = []
        self.peak = 0

    def alloc(self, name, words):
        words = (words + 15) // 16 * 16
        self.live.sort(key=lambda r: r[0])
        pos = 0
        off = None
        for (o, s, _) in self.live:
            if o - pos >= words:
                off = pos
                break
            pos = o + s
        if off is None:
            if self.words - pos >= words:
                off = pos
            else:
                raise MemoryError(f"arena full allocating {name} ({words} words); live="
                                  f"{[(r.name, s) for (_, s, r) in self.live]}")
        pending = []
        for (o, s, ops) in self.freed:
            if o < off + words and off < o + s:
                pending.extend(ops)
        r = Region(self, name, off, words, _compress(pending))
        self.live.append((off, words, r))
        self.peak = max(self.peak, off + words)
        return r

    def free(self, region):
        self.live = [x for x in self.live if x[2] is not region]
        self.freed.append((region.off, region.size, region.collect()))


class Slot:
    def __init__(self, region, key):
        self.region = region
        self.key = key
        self.t = None


class SlotPool:
    def __init__(self, K, name, nslots, words, dma=False):
        self.K = K
        self.name = name
        self.slots = []
        for i in range(nslots):
            key = None
            if dma:
                key = f"{name}{i}"
                K.dma_keys.append(key)
            self.slots.append(Slot(K.A.alloc(f"{name}{i}", words), key))
        self.i = 0

    def next(self):
        s = self.slots[self.i % len(self.slots)]
        self.i += 1
        s.t = s.region.retile()
        return s

    def free(self):
        for s in self.slots:
            self.K.A.free(s.region)


class Sched:
    def __init__(self):
        self.ops = {e: [] for e in ENGS}
        self.dma_cnt = {}

    def op(self, eng, fn, reads=(), writes=(), dma_key=None):
        o = Op(eng, fn)
        o.idx = len(self.ops[eng])
        o.dma_key = dma_key
        deps = []
        for t in reads:
            if t.writer is not None:
                deps.append(t.writer)
        for t in writes:
            if t.writer is not None:
                deps.append(t.writer)
            deps.extend(t.readers)
            deps.extend(t.pending)
        for t in reads:
            t.readers.append(o)
        for t in writes:
            t.writer = o
            t.readers = []
        seen = set()
        for d in deps:
            if d is o or id(d) in seen:
                continue
            seen.add(id(d))
            if d.eng == "pe" and eng == "pe" and d.dma_key is None and dma_key is None:
                continue
            o.deps.append(d)
            d.needed = True
        self.ops[eng].append(o)
        return o

    def emit(self, sems, dma_sems):
        for e in ENGS:
            cnt = 0
            for o in self.ops[e]:
                if o.dma_key is not None:
                    c = self.dma_cnt.get(o.dma_key, 0) + 16
                    self.dma_cnt[o.dma_key] = c
                    o.sig = ("dma:" + o.dma_key, c)
                elif o.needed:
                    cnt += 1
                    o.sig = (e, cnt)

        def run(e, engobj):
            seen = {}
            for o in self.ops[e]:
                for d in o.deps:
                    sname, val = d.sig
                    if seen.get(sname, 0) >= val:
                        continue
                    seen[sname] = val
                    sem = dma_sems[sname[4:]] if sname.startswith("dma:") else sems[sname]
                    engobj.wait_ge(sem, val)
                if o.fn is None:
                    continue
                ins = o.fn(engobj)
                if o.sig is not None:
                    sname, val = o.sig
                    if sname.startswith("dma:"):
                        ins.then_inc(dma_sems[sname[4:]], 16)
                    else:
                        ins.then_inc(sems[sname], 1)
        return run


class K_:
    pass


def mm(K, out, lhsT, rhs, start, stop, reads, writes):
    return K.S.op("pe", lambda e: e.matmul(out, lhsT=lhsT, rhs=rhs, start=start, stop=stop),
                  reads=reads, writes=writes)


def act(K, out, in_, func, reads, writes, scale=1.0, bias=0.0):
    return K.S.op("act", lambda e: e.activation(out=out, in_=in_, func=func, bias=bias, scale=scale),
                  reads=reads, writes=writes)


def tt(K, out, in0, in1, op, reads, writes, eng="dve"):
    return K.S.op(eng, lambda e: e.tensor_tensor(out=out, in0=in0, in1=in1, op=op), reads=reads, writes=writes)


def ts(K, out, in0, s1, op0, reads, writes, s2=None, op1=None, accum=None):
    if op1 is None:
        return K.S.op("dve", lambda e: e.tensor_scalar(out=out, in0=in0, scalar1=s1, scalar2=None, op0=op0),
                      reads=reads, writes=writes)
    if accum is None:
        return K.S.op("dve", lambda e: e.tensor_scalar(out=out, in0=in0, scalar1=s1, scalar2=s2, op0=op0, op1=op1),
                      reads=reads, writes=writes)
    return K.S.op("dve", lambda e: e.tensor_scalar(out=out, in0=in0, scalar1=s1, scalar2=s2, op0=op0, op1=op1,
                                                    accum_out=accum), reads=reads, writes=writes)


def stt(K, out, in0, scalar, in1, op0, op1, reads, writes):
    return K.S.op("dve", lambda e: e.scalar_tensor_tensor(out=out, in0=in0, scalar=scalar, in1=in1, op0=op0, op1=op1),
                  reads=reads, writes=writes)


def wload(K, pool, views):
    s = pool.next()
    for dst, src in views:
        K.S.op("pool", (lambda e, d=dst(s), sr=src: e.dma_start(out=d, in_=sr)), writes=[s.t], dma_key=s.key)
    return s


def w3(slot, kc, cols):
    return slot.region.bf16()[:, :kc * cols].rearrange("p (c f) -> p c f", c=kc)


def wload_cols(K, pool, wview, c0, ncols, kc=8):
    s = wload(K, pool, [(lambda s_: w3(s_, kc, ncols), wview[:, :, c0:c0 + ncols])])
    return w3(s, kc, ncols), s.t


def rmsnorm(K, gidx, tcs, hT, HT, hcol0=0):
    sqp = SlotPool(K, "nsq", 2, 256)
    rsp = SlotPool(K, "nrs", 2, 512)
    for n, tc in enumerate(tcs):
        cs = slice(tc * 512, (tc + 1) * 512)
        hs = slice(tc * 512 - hcol0, (tc + 1) * 512 - hcol0)
        bank, bt = K.ps[6 + n % 2], K.PB[6 + n % 2]
        for c in range(8):
            sq = sqp.next()
            act(K, sq.region.bf16(), K.xT[:, c, cs], ACT.Square, [K.XT[c, tc]], [sq.t])
            mm(K, bank[:], K.c_ones, sq.region.bf16(), c == 0, c == 7, [sq.t, K.TC], [bt])
        rs = rsp.next()
        act(K, rs.region.f32(), bank[:], ACT.Ln, [bt], [rs.t], scale=1.0 / D, bias=K.c_eps[:, 0:1])
        act(K, rs.region.f32(), rs.region.f32(), ACT.Exp, [rs.t], [rs.t], scale=-0.5)
        for c in range(8):
            stt(K, hT[:, c, hs], K.xT[:, c, cs], K.gains[:, gidx, c:c + 1], rs.region.f32(), ALU.mult, ALU.mult,
                [K.XT[c, tc], rs.t, K.TC], [HT[c, tc]])
    sqp.free()
    rsp.free()


def l0_mixer(K):
    S, A = K.S, K.A
    wv = K.dram["ev_w_in"].rearrange("(c p) f -> p c f", p=128)
    wo = K.dram["ev_w_out"].rearrange("(c p) f -> p c f", p=128)
    r_h = A.alloc("hT", 8 * 1024)
    hT = r_h.bf16().rearrange("p (c t) -> p c t", c=8)
    HT = {(c, tc): r_h.tile() for c in range(8) for tc in range(4)}
    rmsnorm(K, 0, range(4), hT, HT)

    wpA = SlotPool(K, "wA", 4, 512, dma=True)
    r_cat = A.alloc("catT", 8 * 1024)
    catT = r_cat.bf16().rearrange("p (c t) -> p c t", c=8)
    CT = {(c, tc): r_cat.tile() for c in range(8) for tc in range(4)}

    r_v = A.alloc("v", 16 * 256)
    vsb = r_v.bf16().rearrange("p (i f) -> p i f", i=16)
    VT = [r_v.tile() for _ in range(16)]
    wpB = SlotPool(K, "wB", 1, 2048, dma=True)
    wvv, t_wvv = wload_cols(K, wpB, wv, 1024, 512)
    for i in range(16):
        bank, bt = K.ps[i % 2], K.PB[i % 2]
        for kc in range(8):
            mm(K, bank[:], hT[:, kc, i * 128:(i + 1) * 128], wvv[:, kc, :], kc == 0, kc == 7, [HT[kc, i // 4], t_wvv], [bt])
        act(K, vsb[:, i, :], bank[:], ACT.Copy, [bt], [VT[i]])
    wpB.free()

    qp = SlotPool(K, "qT", 2, 1024)
    kp = SlotPool(K, "kT", 2, 1024)
    ep = SlotPool(K, "sbe", 2, 512)
    spp = SlotPool(K, "sbsp", 4, 512)
    nlp = SlotPool(K, "sbnl", 3, 256)
    lsp = SlotPool(K, "sbls", 2, 256)
    tmpp = SlotPool(K, "sbtmp", 3, 512)
    wp = SlotPool(K, "sbw", 3, 256)
    PJ = {}

    def make_proj(hp, bank_q, bank_k, eng):
        wq, t_wq = wload_cols(K, wpA, wv, hp * 128, 128)
        wk, t_wk = wload_cols(K, wpA, wv, 512 + hp * 128, 128)
        qs, ks = qp.next(), kp.next()
        d = dict(qT=qs.region.bf16(), kT=ks.region.bf16(),
                 QT=[qs.region.tile() for _ in range(4)], KT=[ks.region.tile() for _ in range(4)])
        PJ[hp] = d
        units = []
        for tc in range(4):
            for (w_, t_w, dstT, TT, sc, bi) in ((wq, t_wq, d["qT"], d["QT"], 0.125, bank_q), (wk, t_wk, d["kT"], d["KT"], 1.0, bank_k)):
                def unit(tc=tc, w_=w_, t_w=t_w, dstT=dstT, TT=TT, sc=sc, bi=bi):
                    cs = slice(tc * 512, (tc + 1) * 512)
                    bank, bt = K.ps[bi], K.PB[bi]
                    for kc in range(8):
                        mm(K, bank[:], w_[:, kc, :], hT[:, kc, cs], kc == 0, kc == 7, [HT[kc, tc], t_w], [bt])
                    if eng == "act":
                        act(K, dstT[:, cs], bank[:], ACT.Copy, [bt], [TT[tc]], scale=sc)
                    else:
                        ts(K, dstT[:, cs], bank[:], sc, ALU.mult, [bt], [TT[tc]])
                units.append(unit)
        return units

    for u_ in make_proj(0, 0, 1, "act"):
        u_()
    for hp in range(4):
        qT, kT, QT, KT = PJ[hp]["qT"], PJ[hp]["kT"], PJ[hp]["QT"], PJ[hp]["KT"]
        nxt_units = make_proj(hp + 1, 7, 7, "dve") if hp < 3 else []
        tiles = []
        for hh in range(2):
            for qc in range(4):
                for b in range(4 * qc + 3, -1, -1):
                    tiles.append((hh, qc, b))
        st = {}

        def stage1(i):
            hh, qc, b = tiles[i]
            pb = hh * 64
            c0 = max(0, b - 4 * qc) * 128
            n = 512 - c0
            diag = b >= 4 * qc
            zb, zt = K.ps[i % 3], K.PB[i % 3]
            mm(K, zb[:, 0:n], kT[pb:pb + 64, b * 128:(b + 1) * 128], qT[pb:pb + 64, qc * 512 + c0:(qc + 1) * 512],
               True, True, [KT[b // 4], QT[qc]], [zt])
            e_, sp_ = ep.next(), spp.next()
            act(K, e_.region.f32()[:, 0:n], zb[:, 0:n], ACT.Exp, [zt], [e_.t], scale=-1.0)
            act(K, sp_.region.f32()[:, 0:n], e_.region.f32()[:, 0:n], ACT.Ln, [e_.t], [sp_.t], bias=K.c_one[:, 0:1])
            st[i] = dict(sp=sp_, n=n, c0=c0, diag=diag, zb=zb, zt=zt)

        def stage1b(i):
            d = st[i]
            n, zb, zt, sp_ = d["n"], d["zb"], d["zt"], d["sp"]
            nl_ = nlp.next()
            tt(K, nl_.region.bf16()[:, 0:n], zb[:, 0:n], sp_.region.f32()[:, 0:n], ALU.add, [zt, sp_.t], [nl_.t])
            if d["diag"]:
                tt(K, nl_.region.bf16()[:, 0:128], nl_.region.bf16()[:, 0:128], K.c_tri, ALU.mult, [nl_.t, K.TC], [nl_.t])
            d["nl"] = nl_

        def stage2(i):
            hh, qc, b = tiles[i]
            d = st[i]
            n, c0 = d["n"], d["c0"]
            first = b == 4 * qc + 3
            ab, at = K.ps[3 + i % 2], K.PB[3 + i % 2]
            nl = d["nl"].region.bf16()
            if first:
                la, lb = lsp.next(), lsp.next()
                S.op("pool", (lambda e, r=la: e.memset(r.region.bf16(), 0.0)), writes=[la.t])
                S.op("pool", (lambda e, r=lb: e.memset(r.region.bf16(), 0.0)), writes=[lb.t])
                st["ls"] = [la, lb]
                st["lsi"] = 0
            cur = st["ls"][st["lsi"] % 2]
            nxt = st["ls"][(st["lsi"] + 1) % 2]
            mm(K, ab[:, 0:n], K.c_ustrict, nl[:, 0:n], True, first, [d["nl"].t, K.TC], [at])
            if not first:
                mm(K, ab[:, 0:n], K.c_ones, cur.region.bf16()[:, c0:512], False, True, [cur.t, K.TC], [at])
            if b > 0:
                if c0 > 0:
                    pass
                tt(K, nxt.region.bf16()[:, c0:512], cur.region.bf16()[:, c0:512], nl[:, 0:n], ALU.add,
                   [cur.t, d["nl"].t], [nxt.t], eng="pool")
                st["lsi"] += 1
            tm = tmpp.next()
            tt(K, tm.region.f32()[:, 0:n], ab[:, 0:n], d["sp"].region.f32()[:, 0:n], ALU.add, [at, d["sp"].t], [tm.t])
            d["tm"] = tm

        def stage2b(i):
            d = st[i]
            n, tm = d["n"], d["tm"]
            w_ = wp.next()
            act(K, w_.region.bf16()[:, 0:n], tm.region.f32()[:, 0:n], ACT.Exp, [tm.t], [w_.t], scale=-1.0)
            if d["diag"]:
                tt(K, w_.region.bf16()[:, 0:128], w_.region.bf16()[:, 0:128], K.c_tri, ALU.mult, [w_.t, K.TC], [w_.t], eng="pool")
            d["w"] = w_

        def stage3(i):
            hh, qc, b = tiles[i]
            d = st[i]
            n, c0 = d["n"], d["c0"]
            pb = hh * 64
            h = hp * 2 + hh
            ob, ot = K.ps[5 + qc % 2], K.PB[5 + qc % 2]
            first = b == 4 * qc + 3
            w_ = d["w"].region.bf16()
            lhs = vsb[:, b, h * 64:(h + 1) * 64]
            pieces = [(0, n)] if not d["diag"] or n == 128 else [(0, 128), (128, n)]
            for pi, (a0, a1) in enumerate(pieces):
                mm(K, ob[pb:pb + 64, c0 + a0:c0 + a1], lhs, w_[:, a0:a1], first and pi == 0,
                   b == 0 and pi == len(pieces) - 1, [VT[b], d["w"].t], [ot])
            if b == 0:
                K.S.op("dve", (lambda e, o=ob, p=pb, q=qc, hp=hp: e.tensor_copy(out=catT[p:p + 64, hp, q * 512:(q + 1) * 512],
                                                                       in_=o[p:p + 64, :])),
                       reads=[ot], writes=[CT[hp, qc]])
            del st[i]

        N = len(tiles)
        for i in range(N + 4):
            if i < N:
                stage1(i)
            if 0 <= i - 1 < N:
                stage1b(i - 1)
            if 0 <= i - 2 < N:
                stage2(i - 2)
            if 0 <= i - 3 < N:
                stage2b(i - 3)
            if 0 <= i - 4 < N:
                stage3(i - 4)
            if nxt_units and i >= 30 and (i - 30) % 6 == 0:
                nxt_units.pop(0)()
        while nxt_units:
            nxt_units.pop(0)()
    for p_ in (qp, kp, ep, spp, nlp, lsp, tmpp, wp):
        p_.free()
    A.free(r_v)

    r_g = A.alloc("convg", 2064)
    g = r_g.f32()
    GT = [r_g.tile() for _ in range(5)]
    up_ = SlotPool(K, "cvu", 2, 512)
    y1p = SlotPool(K, "cvy", 2, 512)
    for cc in range(4):
        wb, t_wb = wload_cols(K, wpA, wv, 1536 + cc * 128, 128)
        wc, t_wc = wload_cols(K, wpA, wv, 2048 + cc * 128, 128)
        wu, t_wu = wload_cols(K, wpA, wv, 2560 + cc * 128, 128)
        S.op("dve", lambda e: e.memset(g[:, 0:16], 0.0), writes=[GT[4]])
        for tc in range(4):
            cs = slice(tc * 512, (tc + 1) * 512)
            banks = []
            for j, (w_, t_w) in enumerate(((wb, t_wb), (wc, t_wc), (wu, t_wu))):
                bi = (tc % 2) * 3 + j
                bank, bt = K.ps[bi], K.PB[bi]
                for kc in range(8):
                    mm(K, bank[:], w_[:, kc, :], hT[:, kc, cs], kc == 0, kc == 7, [HT[kc, tc], t_w], [bt])
                banks.append((bank, bt))
            u_ = up_.next()
            act(K, u_.region.f32(), banks[2][0][:], ACT.Copy, [banks[2][1]], [u_.t])
            tt(K, g[:, 16 + tc * 512:16 + (tc + 1) * 512], banks[1][0][:], u_.region.f32(), ALU.mult,
               [banks[1][1], u_.t], [GT[tc]])
            prev = GT[tc - 1] if tc > 0 else GT[4]
            y = y1p.next()
            gs = lambda sh: g[:, 16 + tc * 512 - sh:16 + (tc + 1) * 512 - sh]
            ts(K, y.region.f32(), gs(2), K.convw[:, cc, 0:1], ALU.mult, [GT[tc], prev, K.TC], [y.t])
            stt(K, y.region.f32(), gs(1), K.convw[:, cc, 1:2], y.region.f32(), ALU.mult, ALU.add, [GT[tc], prev, y.t, K.TC], [y.t])
            stt(K, y.region.f32(), gs(0), K.convw[:, cc, 2:3], y.region.f32(), ALU.mult, ALU.add, [GT[tc], y.t, K.TC], [y.t])
            tt(K, catT[:, 4 + cc, cs], banks[0][0][:], y.region.f32(), ALU.mult, [banks[0][1], y.t], [CT[4 + cc, tc]])
    up_.free()
    y1p.free()
    A.free(r_g)
    A.free(r_h)

    for dc in range(8):
        wob, t_wo = wload_cols(K, wpA, wo, dc * 128, 128)
        for tc in range(4):
            cs = slice(tc * 512, (tc + 1) * 512)
            bank, bt = K.ps[(dc * 4 + tc) % 4], K.PB[(dc * 4 + tc) % 4]
            for kc in range(8):
                mm(K, bank[:], wob[:, kc, :], catT[:, kc, cs], kc == 0, kc == 7, [CT[kc, tc], t_wo], [bt])
            tt(K, K.xT[:, dc, cs], bank[:], K.xT[:, dc, cs], ALU.add, [bt, K.XT[dc, tc]], [K.XT[dc, tc]])
    wpA.free()
    A.free(r_cat)


def ffn(K, layer, store_out=None):
    S, A = K.S, K.A
    wg = K.dram["ffn_w_gate"][layer].rearrange("(c p) f -> p c f", p=128)
    wu = K.dram["ffn_w_up"][layer].rearrange("(c p) f -> p c f", p=128)
    wd = K.dram["ffn_w_down"][layer].rearrange("(c p) f -> p c f", p=128)
    wpG = SlotPool(K, "wG", 4, 1024, dma=True)
    wpD = SlotPool(K, "wD", 2, 1408, dma=True)
    sgp = SlotPool(K, "sg", 2, 512)
    r_hs, hTs, HTs = [], [], []
    for half in range(2):
        r_h = A.alloc("hTf", 8 * 512)
        r_hs.append(r_h)
        hTs.append(r_h.bf16().rearrange("p (c t) -> p c t", c=8))
        HTs.append({(c, tc): r_h.tile() for c in range(8) for tc in (2 * half, 2 * half + 1)})
    rmsnorm(K, layer * 2 + 1, (0, 1), hTs[0], HTs[0], hcol0=0)
    for half in range(2):
        r_h, hT, HT = r_hs[half], hTs[half], HTs[half]
        r_g = A.alloc("gT", NFC * 512)
        gT = r_g.bf16().rearrange("p (c t) -> p c t", c=NFC)
        GT = {(fc, j): r_g.tile() for fc in range(NFC) for j in range(2)}
        n = 0
        for f2 in range(NFC // 2):
            wgb, t_wg = wload_cols(K, wpG, wg, f2 * 256, 256)
            wub, t_wu = wload_cols(K, wpG, wu, f2 * 256, 256)
            for fi in range(2):
                fc = f2 * 2 + fi
                for j in range(2):
                    tc = 2 * half + j
                    hs = slice(j * 512, (j + 1) * 512)
                    gb, gt_ = K.ps[(n % 2) * 2], K.PB[(n % 2) * 2]
                    ub, ut_ = K.ps[(n % 2) * 2 + 1], K.PB[(n % 2) * 2 + 1]
                    n += 1
                    for kc in range(8):
                        mm(K, gb[:], wgb[:, kc, fi * 128:(fi + 1) * 128], hT[:, kc, hs], kc == 0, kc == 7, [HT[kc, tc], t_wg], [gt_])
                    for kc in range(8):
                        mm(K, ub[:], wub[:, kc, fi * 128:(fi + 1) * 128], hT[:, kc, hs], kc == 0, kc == 7, [HT[kc, tc], t_wu], [ut_])
                    sg = sgp.next()
                    act(K, sg.region.f32(), gb[:], ACT.Silu, [gt_], [sg.t])
                    tt(K, gT[:, fc, hs], ub[:], sg.region.f32(), ALU.mult, [ut_, sg.t], [GT[fc, j]])
        A.free(r_h)
        if half == 0:
            rmsnorm(K, layer * 2 + 1, (2, 3), hTs[1], HTs[1], hcol0=1024)
        for dc in range(8):
            wdb, t_wd = wload_cols(K, wpD, wd, dc * 128, 128, kc=NFC)
            for j in range(2):
                tc = 2 * half + j
                hs = slice(j * 512, (j + 1) * 512)
                cs = slice(tc * 512, (tc + 1) * 512)
                bank, bt = K.ps[4 + (dc * 2 + j) % 4], K.PB[4 + (dc * 2 + j) % 4]
                for kc in range(NFC):
                    mm(K, bank[:], wdb[:, kc, :], gT[:, kc, hs], kc == 0, kc == NFC - 1, [GT[kc, j], t_wd], [bt])
                tt(K, K.xT[:, dc, cs], bank[:], K.xT[:, dc, cs], ALU.add, [bt, K.XT[dc, tc]], [K.XT[dc, tc]])
            if store_out is not None:
                hs2 = slice(half * 1024, (half + 1) * 1024)
                store_out.append(S.op("sp", (lambda e, dc=dc, hs2=hs2: e.dma_start(out=K.yv[:, dc, hs2], in_=K.xT[:, dc, hs2])),
                                      reads=[K.XT[dc, 2 * half], K.XT[dc, 2 * half + 1]], dma_key="out"))
        A.free(r_g)
    wpG.free()
    wpD.free()
    sgp.free()


QE = [0, 1, 2, 3, 8, 9, 10, 11]
QO = [4, 5, 6, 7, 12, 13, 14, 15]


def RECIP(e, out, in_):
    if USE_FAST_RECIP:
        return e.reciprocal_approx_fast(out=out, in_=in_)
    return e.reciprocal(out=out, in_=in_)


def l1_mixer(K):
    S, A = K.S, K.A
    wv = K.dram["od_w_in"].rearrange("(c p) f -> p c f", p=128)
    wo = K.dram["od_w_out"].rearrange("(c p) f -> p c f", p=128)
    r_h = A.alloc("hT1", 8 * 1024)
    hT = r_h.bf16().rearrange("p (c t) -> p c t", c=8)
    HT = {(c, tc): r_h.tile() for c in range(8) for tc in range(4)}
    rmsnorm(K, 2, range(4), hT, HT)

    wpA = SlotPool(K, "wA", 3, 512, dma=True)
    r_q = [A.alloc(f"q1_{tc}", 8 * 256) for tc in range(4)]
    qTc = [r.bf16().rearrange("p (c t) -> p c t", c=8) for r in r_q]
    QT = {(c, tc): r_q[tc].tile() for c in range(8) for tc in range(4)}
    r_k = A.alloc("k1", 2 * 1024)
    kT = r_k.bf16().rearrange("p (c t) -> p c t", c=2)
    KT = {(c, tc): r_k.tile() for c in range(2) for tc in range(4)}
    r_qi = [A.alloc(f"qi1_{tc}", 4 * 256) for tc in range(4)]
    qiTc = [r.bf16().rearrange("p (c t) -> p c t", c=4) for r in r_qi]
    QIT = {(c, tc): r_qi[tc].tile() for c in range(4) for tc in range(4)}
    r_ki = A.alloc("ki1", 1024)
    kiT = r_ki.bf16()
    KIT = [r_ki.tile() for _ in range(4)]
    r_v = A.alloc("v1", 16 * 128)
    vsb = r_v.bf16().rearrange("p (i f) -> p i f", i=16)
    VT = [r_v.tile() for _ in range(16)]
    r_wi = A.alloc("wi1", 128)
    wi = r_wi.f32().rearrange("p (i h) -> p i h", i=16)
    WIT = [r_wi.tile() for _ in range(16)]

    wpB = SlotPool(K, "wB", 1, 8 * 264 // 2, dma=True)
    sB = wload(K, wpB, [(lambda s_: w3(s_, 8, 264)[:, :, 0:256], wv[:, :, 1280:1536]),
                        (lambda s_: w3(s_, 8, 264)[:, :, 256:264], wv[:, :, 2112:2120])])
    wB = w3(sB, 8, 264)
    for i in range(16):
        bank, bt = K.ps[i % 2], K.PB[i % 2]
        for kc in range(8):
            mm(K, bank[:, 0:264], hT[:, kc, i * 128:(i + 1) * 128], wB[:, kc, :], kc == 0, kc == 7, [HT[kc, i // 4], sB.t], [bt])
        act(K, vsb[:, i, :], bank[:, 0:256], ACT.Copy, [bt], [VT[i]])
        K.S.op("dve", (lambda e, i=i, bank=bank: e.tensor_copy(out=wi[:, i, :], in_=bank[:, 256:264])), reads=[bt], writes=[WIT[i]])
    wpB.free()

    sqp = SlotPool(K, "qsq", 2, 256)
    rsp = SlotPool(K, "qrs", 2, 512)
    jobs = []
    for j in range(8):
        jobs.append(("q", j, [(0, QE[j] * 64), (64, QO[j] * 64)]))
    for j in range(2):
        jobs.append(("k", j, [(0, 1024 + j * 128), (64, 1024 + j * 128 + 64)]))
    for j in range(4):
        jobs.append(("qi", j, [(0, 1536 + j * 128), (64, 1536 + j * 128 + 64)]))
    jobs.append(("ki", 0, [(0, 2048), (64, 2048)]))
    nb = 0
    for kind, j, cols in jobs:
        s = wload(K, wpA, [((lambda s_, o=o: w3(s_, 8, 128)[:, :, o:o + 64]), wv[:, :, c:c + 64]) for (o, c) in cols])
        wblk = w3(s, 8, 128)
        for tc in range(4):
            cs = slice(tc * 512, (tc + 1) * 512)
            bank, bt = K.ps[nb % 3], K.PB[nb % 3]
            nb += 1
            for kc in range(8):
                mm(K, bank[:], wblk[:, kc, :], hT[:, kc, cs], kc == 0, kc == 7, [HT[kc, tc], s.t], [bt])
            if kind in ("q", "k"):
                sq = sqp.next()
                act(K, sq.region.bf16(), bank[:], ACT.Square, [bt], [sq.t])
                b2, bt2 = K.ps[3 + nb % 2], K.PB[3 + nb % 2]
                mm(K, b2[:], K.c_blk, sq.region.bf16(), True, True, [sq.t, K.TC], [bt2])
                rs = rsp.next()
                if kind == "q":
                    act(K, rs.region.f32(), b2[:], ACT.Ln, [bt2], [rs.t], scale=1.0, bias=K.c_eps[:, 1:2])
                else:
                    act(K, rs.region.f32(), b2[:], ACT.Ln, [bt2], [rs.t], scale=1.0 / 64, bias=K.c_eps[:, 0:1])
                act(K, rs.region.f32(), rs.region.f32(), ACT.Exp, [rs.t], [rs.t], scale=-0.5)
                if kind == "q":
                    stt(K, qTc[tc][:, j, :], bank[:], K.qkg[:, 0:1], rs.region.f32(), ALU.mult, ALU.mult, [bt, rs.t, K.TC], [QT[j, tc]])
                else:
                    stt(K, kT[:, j, cs], bank[:], K.qkg[:, 1:2], rs.region.f32(), ALU.mult, ALU.mult, [bt, rs.t, K.TC], [KT[j, tc]])
            elif kind == "qi":
                act(K, qiTc[tc][:, j, :], bank[:], ACT.Copy, [bt], [QIT[j, tc]])
            else:
                act(K, kiT[:, cs], bank[:], ACT.Copy, [bt], [KIT[tc]])
    sqp.free()
    rsp.free()
    A.free(r_h)

    r_db = A.alloc("dbT", 16 * 128)
    dbT = r_db.bf16().rearrange("p (h u) -> p h u", h=16)
    t_db = r_db.tile()
    r_bt = A.alloc("biasT", 16 * 256)
    t_bt = r_bt.tile()
    K.dma_keys.append("biasT")
    S.op("sp", lambda e: e.dma_start(out=r_bt.f32(), in_=K.dram["biasT"]), writes=[t_bt], dma_key="biasT")
    for h in range(16):
        ts(K, dbT[:, h, :], r_bt.f32()[:, h * 256:(h + 1) * 256], K.c31[:, h:h + 1], ALU.subtract, [t_bt, K.TC], [t_db])
    A.free(r_bt)

    FP8 = mybir.dt.float8e4
    r_nmt = [A.alloc("notMT0", 16 * 128), A.alloc("notMT1", 16 * 128)]
    nmtv = [r.f32().bitcast(FP8).rearrange("p (b t) -> p b t", b=16) for r in r_nmt]
    NMTS = {}

    def score_gen(qc):
        notMT = nmtv[qc % 2]
        NMT = [r_nmt[qc % 2].retile() if b == 0 else r_nmt[qc % 2].tile() for b in range(16)]
        NMTS[qc] = NMT
        if qc == 0:
            S.op("dve", lambda e: e.memset(notMT[:, 0:2, 0:256], 0.0), writes=NMT[0:2])
            for b in range(2):
                K.S.op("dve", (lambda e, b=b: e.tensor_copy(out=notMT[:, b, b * 128:(b + 1) * 128], in_=K.c_ncT, saturate=False)),
                       reads=[K.TC], writes=[NMT[b]])
        rp = SlotPool(K, "relu", 2, 512)
        smp = SlotPool(K, "bis", 4, 16)
        tmp_regions = []
        yield 1.0
        for tl in [[i for i in range(4 * qc, 4 * qc + 4) if i >= 2]]:
            info = {}
            chunks = []
            for i in tl:
                sc = Slot(A.alloc(f"sc{i}", 128 * (i + 1)), None)
                tmp_regions.append(sc.region)
                info[i] = dict(sc=sc, n=128 * (i + 1), tiles={})
                for k4 in range((128 * (i + 1) + 511) // 512):
                    ncols = min(512, 128 * (i + 1) - 512 * k4)
                    info[i]["tiles"][k4] = sc.region.tile()
                    chunks.append((i, k4, ncols))
            nb = 0
            for h in range(8):
                pb = (h % 2) * 64
                for (i, k4, ncols) in chunks:
                    sc = info[i]["sc"].region.f32()
                    stile = info[i]["tiles"][k4]
                    bank, bt = K.ps[6 + nb % 2], K.PB[6 + nb % 2]
                    nb += 1
                    mm(K, bank[:, 0:ncols], qiTc[i // 4][pb:pb + 64, h // 2, (i % 4) * 128:(i % 4 + 1) * 128],
                       kiT[pb:pb + 64, k4 * 512:k4 * 512 + ncols], True, True,
                       [QIT[h // 2, i // 4], KIT[k4]], [bt])
                    r_ = rp.next()
                    act(K, r_.region.f32()[:, 0:ncols], bank[:, 0:ncols], ACT.Relu, [bt], [r_.t])
                    dst = sc[:, k4 * 512:k4 * 512 + ncols]
                    if h == 0:
                        ts(K, dst, r_.region.f32()[:, 0:ncols], wi[:, i, 0:1], ALU.mult, [r_.t, WIT[i]], [stile])
                    else:
                        stt(K, dst, r_.region.f32()[:, 0:ncols], wi[:, i, h:h + 1], dst, ALU.mult, ALU.add,
                            [r_.t, WIT[i], stile], [stile])
                    yield 0.7
            bis = {}
            for i in tl:
                sm = smp.next()
                sc = info[i]["sc"].region.f32()
                n = info[i]["n"]
                allt = list(info[i]["tiles"].values())
                v = sm.region.f32()
                K.S.op("dve", (lambda e, v=v, sc=sc, n=n: e.tensor_reduce(out=v[:, 0:1], in_=sc[:, 0:n], axis=AX.X, op=ALU.max)),
                       reads=allt, writes=[sm.t])
                K.S.op("dve", (lambda e, v=v, sc=sc, n=n: e.tensor_reduce(out=v[:, 1:2], in_=sc[:, 0:n], axis=AX.X, op=ALU.min)),
                       reads=allt, writes=[sm.t])
                tt(K, v[:, 2:3], v[:, 0:1], v[:, 1:2], ALU.subtract, [sm.t], [sm.t])
                tt(K, sc[:, i * 128:(i + 1) * 128], sc[:, i * 128:(i + 1) * 128], K.c_negmask, ALU.add,
                   allt + [K.TC], allt)
                nm = Slot(A.alloc(f"nm{i}", 64 * (i + 1)), None)
                nm.t = nm.region.tile()
                tmp_regions.append(nm.region)
                if i % 2 == 0:
                    ts(K, v[:, 6:7], v[:, 1:2], -1.0, ALU.mult, [sm.t], [sm.t])
                bis[i] = (sm, v, sc, n, allt, nm)
                yield 1.0
            for it in range(1, NBIS + 1):
                f = 2.0 ** (-it)
                for i in tl:
                    sm, v, sc, n, allt, nm = bis[i]
                    junk = nm.region.bf16()
                    if i % 2 == 0:
                        stt(K, v[:, 3:4], v[:, 2:3], -f, v[:, 6:7], ALU.mult, ALU.add, [sm.t], [sm.t])
                        K.S.op("act", (lambda e, junk=junk, sc=sc, n=n, v=v: e.activation(
                            out=junk[:, 0:n], in_=sc[:, 0:n], func=ACT.Sign, bias=v[:, 3:4], scale=1.0, accum_out=v[:, 4:5])),
                            reads=allt + [sm.t], writes=[sm.t, nm.t])
                        ts(K, v[:, 5:6], v[:, 4:5], 2.0 * TOPK - 1.0 - n, ALU.is_ge, [sm.t], [sm.t], s2=-f, op1=ALU.mult)
                        stt(K, v[:, 6:7], v[:, 5:6], v[:, 2:3], v[:, 6:7], ALU.mult, ALU.add, [sm.t], [sm.t])
                    else:
                        stt(K, v[:, 3:4], v[:, 2:3], f, v[:, 1:2], ALU.mult, ALU.add, [sm.t], [sm.t])
                        ts(K, junk[:, 0:n], sc[:, 0:n], v[:, 3:4], ALU.is_ge, allt + [sm.t], [sm.t], s2=0.0, op1=ALU.add, accum=v[:, 4:5])
                        ts(K, v[:, 5:6], v[:, 4:5], TOPK - 0.5, ALU.is_ge, [sm.t], [sm.t], s2=f, op1=ALU.mult)
                        stt(K, v[:, 1:2], v[:, 5:6], v[:, 2:3], v[:, 1:2], ALU.mult, ALU.add, [sm.t], [sm.t])
                yield 1.0 + sum(bis[i][3] for i in tl if i % 2 == 1) / 1000.0
            for i in tl:
                if i % 2 == 0:
                    ts(K, bis[i][1][:, 1:2], bis[i][1][:, 6:7], -1.0, ALU.mult, [bis[i][0].t], [bis[i][0].t])
            for i in tl:
                sm, v, sc, n, allt, nm = bis[i]
                ts(K, nm.region.bf16()[:, 0:n], sc[:, 0:n], v[:, 1:2], ALU.is_lt, allt + [sm.t], [nm.t])
            yield 1.5
            for b in range(tl[-1] + 1):
                ii = [i for i in tl if i >= b]
                bank, bt = K.ps[6 + b % 2], K.PB[6 + b % 2]
                bv = bank[:].bitcast(BF16)
                for x, i in enumerate(ii):
                    nm = bis[i][5]
                    K.S.op("pe", (lambda e, bv=bv, x=x, nm=nm, b=b: e.transpose(out=bv[:, x * 128:(x + 1) * 128],
                                                                              in_=nm.region.bf16()[:, b * 128:(b + 1) * 128],
                                                                              identity=K.c_ident)),
                           reads=[nm.t, K.TC], writes=[bt])
                c_lo = (ii[0] - 4 * qc) * 128
                K.S.op("act", (lambda e, o=notMT[:, b, c_lo:c_lo + 128 * len(ii)], i_=bv[:, 0:128 * len(ii)]:
                               e.activation(out=o, in_=i_, func=ACT.Copy, saturate=False)), reads=[bt], writes=[NMT[b]])
                yield 0.5
        for p_ in (rp, smp):
            p_.free()
        for r in tmp_regions:
            A.free(r)
        A.free(r_qi[qc])

    def score_units(qc):
        tl = [i for i in range(4 * qc, 4 * qc + 4) if i >= 2]
        nch = sum((128 * (i + 1) + 511) // 512 for i in tl)
        ndve = sum(128 * (i + 1) for i in tl if i % 2 == 1)
        return 1.0 + 8 * nch * 0.7 + len(tl) * 1.0 + NBIS * (1.0 + ndve / 1000.0) + 1.5 + (tl[-1] + 1) * 0.5

    def attn_gen(qc):
        notMT = nmtv[qc % 2]
        NMT = NMTS[qc]
        r_o = A.alloc("oT", 8 * 256)
        oT = r_o.bf16().rearrange("p (c t) -> p c t", c=8)
        OT = [r_o.tile() for _ in range(8)]
        pp = SlotPool(K, "pexp", 3, 256)
        rdp = SlotPool(K, "rden", 1, 512)
        tiles = []
        for j in range(8):
            for hh in range(2):
                for b in range(4 * qc + 4):
                    tiles.append((j, hh, b))
        st = {}

        def a1(i):
            j, hh, b = tiles[i]
            pb = hh * 64
            h = QE[j] if hh == 0 else QO[j]
            c0 = max(0, b - 4 * qc) * 128
            n = 512 - c0
            lb, lt = K.ps[i % 3], K.PB[i % 3]
            ulo = 512 * qc + c0 - 128 * b
            nbias = max(0, min(256 - ulo, n)) if ulo < 256 else 0
            mm(K, lb[:, 0:n], kT[pb:pb + 64, j // 4, b * 128:(b + 1) * 128], qTc[qc][pb:pb + 64, j, c0:512],
               True, False, [KT[j // 4, b // 4], QT[j, qc]], [lt])
            if nbias > 0:
                mm(K, lb[:, 0:nbias], K.c_ident, dbT[:, h, ulo:ulo + nbias], False, False, [t_db, K.TC], [lt])
            mm(K, lb[:, 0:n], K.c_negI, notMT[:, b, c0:512], False, True, [NMT[b], K.TC], [lt])
            st[i] = [None, n, c0, h, lb, lt]

        def a1b(i):
            _, n, c0, h, lb, lt = st[i]
            p_ = pp.next()
            act(K, p_.region.bf16()[:, 0:n], lb[:, 0:n], ACT.Exp, [lt, K.TC], [p_.t], bias=K.c31[:, h:h + 1])
            st[i][0] = p_

        def a2(i):
            j, hh, b = tiles[i]
            p_, n, c0, h, _lb, _lt = st.pop(i)
            g = h // 4
            hidx = (j * 2 + hh)
            ob, ot = K.ps[3 + hidx % 2], K.PB[3 + hidx % 2]
            last = b == 4 * qc + 3
            pr = p_.region.bf16()[:, 0:n]
            mm(K, ob[0:64, c0:512], vsb[:, b, g * 64:(g + 1) * 64], pr, b == 0, last, [VT[b], p_.t], [ot])
            mm(K, ob[64:128, c0:512], K.c_ones[:, 0:64], pr, b == 0, last, [p_.t, K.TC], [ot])
            if last:
                def fin(ob=ob, ot=ot, h=h):
                    rd = rdp.next()
                    act(K, rd.region.f32()[64:128, :], ob[64:128, :], ACT.Ln, [ot], [rd.t])
                    act(K, rd.region.f32()[64:128, :], rd.region.f32()[64:128, :], ACT.Exp, [rd.t], [rd.t], scale=-1.0)
                    oc, opb = h // 2, (h % 2) * 64
                    tt(K, oT[opb:opb + 64, oc, :], ob[0:64, :], rd.region.f32()[64:128, :], ALU.mult, [ot, rd.t], [OT[oc]])
                pend.append((i + 3, fin))

        N = len(tiles)
        pend = []
        for i in range(N + 6):
            if i < N:
                a1(i)
            if 0 <= i - 1 < N:
                a1b(i - 1)
            if 0 <= i - 2 < N:
                a2(i - 2)
            while pend and pend[0][0] <= i - 2:
                pend.pop(0)[1]()
            yield 0.7
        assert not pend
        pp.free()
        rdp.free()
        for dc in range(8):
            wob, t_wo = wload_cols(K, wpA, wo, dc * 128, 128)
            bank, bt = K.ps[5], K.PB[5]
            for kc in range(8):
                mm(K, bank[:], wob[:, kc, :], oT[:, kc, :], kc == 0, kc == 7, [OT[kc], t_wo], [bt])
            cs = slice(qc * 512, (qc + 1) * 512)
            tt(K, K.xT[:, dc, cs], bank[:], K.xT[:, dc, cs], ALU.add, [bt, K.XT[dc, qc]], [K.XT[dc, qc]])
            yield 1.0
        A.free(r_o)
        A.free(r_q[qc])

    def attn_units(qc):
        return (16 * (4 * qc + 4) + 6) * 0.7 + 8 * 1.0

    for _ in score_gen(0):
        pass
    for qc in range(4):
        ga, na = attn_gen(qc), attn_units(qc)
        gs, ns = (score_gen(qc + 1), score_units(qc + 1)) if qc < 3 else (None, 1)
        da = ds = 0.0
        a_alive, s_alive = True, gs is not None
        while a_alive or s_alive:
            if a_alive and (not s_alive or da * ns <= ds * na):
                try:
                    da += next(ga)
                except StopIteration:
                    a_alive = False
            elif s_alive:
                try:
                    ds += next(gs)
                except StopIteration:
                    s_alive = False
    wpA.free()
    for r in (r_k, r_ki, r_v, r_wi, r_db, r_nmt[0], r_nmt[1]):
        A.free(r)


CONST_F32 = {"trimask": (128, 128), "negmask": (128, 128), "gains": (128, 32), "convw": (128, 12),
             "qkg": (128, 2), "c31": (128, 16), "eps": (128, 2), "one": (128, 1)}
CONST_BF = {"ident": (128, 128), "ustrict": (128, 128), "ones": (128, 128), "blk": (128, 128),
            "ncT": (128, 128), "negI": (128, 128)}
ARENA_WORDS = 53200


def build(parts):
    nc = bass.Bass("TRN2", target_bir_lowering=False)
    K = K_()
    K.nc = nc
    dram = {}
    dram["xT"] = nc.dram_tensor("xT", [D, S_LEN], F32, kind="ExternalInput").ap()
    dram["yT"] = nc.dram_tensor("yT", [D, S_LEN], F32, kind="ExternalOutput").ap()
    shapes = {"ev_w_in": [D, 3072], "ev_w_out": [D, D], "od_w_in": [D, 2120], "od_w_out": [D, D],
              "ffn_w_gate": [2, D, FF], "ffn_w_up": [2, D, FF], "ffn_w_down": [2, FF, D], "biasT": [128, 16 * 256]}
    for k, shp in shapes.items():
        dram[k] = nc.dram_tensor(k, shp, F32, kind="ExternalInput").ap()
    for k, shp in CONST_F32.items():
        dram["c_" + k] = nc.dram_tensor("c_" + k, list(shp), F32, kind="ExternalInput").ap()
    for k, shp in CONST_BF.items():
        dram["c_" + k] = nc.dram_tensor("c_" + k, list(shp), BF16, kind="ExternalInput").ap()
    K.dram = dram
    K.S = Sched()
    K.dma_keys = []
    with ExitStack() as es:
        arena_t = es.enter_context(nc.sbuf_tensor("arena", [128, ARENA_WORDS], F32))
        K.ps = [es.enter_context(nc.psum_tensor(f"ps{i}", [128, 512], F32)) for i in range(8)]
        K.PB = [T(f"pb{i}") for i in range(8)]
        K.A = Arena(arena_t[:], ARENA_WORDS)
        A, S = K.A, K.S
        K.TC = T("consts")
        cw = sum(s[1] for s in CONST_F32.values()) + sum(s[1] // 2 for s in CONST_BF.values())
        r_c = A.alloc("consts", cw)
        off = 0
        cap = {}
        for k, shp in CONST_F32.items():
            ap = r_c.f32()[:, off:off + shp[1]]
            key = "c_" + k
            K.dma_keys.append(key)
            S.op("sp", (lambda e, ap=ap, key=key: e.dma_start(out=ap, in_=dram[key])), writes=[K.TC], dma_key=key)
            cap[k] = ap
            off += shp[1]
        for k, shp in CONST_BF.items():
            ap = r_c.f32()[:, off:off + shp[1] // 2].bitcast(BF16)
            key = "c_" + k
            K.dma_keys.append(key)
            S.op("sp", (lambda e, ap=ap, key=key: e.dma_start(out=ap, in_=dram[key])), writes=[K.TC], dma_key=key)
            cap[k] = ap
            off += shp[1] // 2
        K.c_tri, K.c_negmask = cap["trimask"], cap["negmask"]
        K.gains = cap["gains"].rearrange("p (n c) -> p n c", n=4)
        K.convw = cap["convw"].rearrange("p (c w) -> p c w", c=4)
        K.qkg, K.c31, K.c_eps, K.c_one = cap["qkg"], cap["c31"], cap["eps"], cap["one"]
        K.c_ident, K.c_ustrict, K.c_ones, K.c_blk = cap["ident"], cap["ustrict"], cap["ones"], cap["blk"]
        K.c_ncT, K.c_negI = cap["ncT"], cap["negI"]
        r_x = A.alloc("xT", 8 * S_LEN)
        K.xT = r_x.f32().rearrange("p (c t) -> p c t", c=8)
        K.XT = {(c, tc): r_x.tile() for c in range(8) for tc in range(4)}
        xv = dram["xT"].rearrange("(c p) t -> p c t", p=128)
        for tc in range(4):
            key = f"x{tc}"
            K.dma_keys.append(key)
            S.op("sp", (lambda e, tc=tc: e.dma_start(out=K.xT[:, :, tc * 512:(tc + 1) * 512], in_=xv[:, :, tc * 512:(tc + 1) * 512])),
                 writes=[K.XT[c, tc] for c in range(8)], dma_key=key)
        if "l0mix" in parts:
            l0_mixer(K)
        if "l0ffn" in parts:
            ffn(K, 0)
        if "l1mix" in parts:
            l1_mixer(K)
        K.yv = dram["yT"].rearrange("(c p) t -> p c t", p=128)
        K.dma_keys.append("out")
        outs = []
        if "l1ffn" in parts:
            ffn(K, 1, store_out=outs)
        else:
            for c in range(8):
                outs.append(S.op("sp", (lambda e, c=c: e.dma_start(out=K.yv[:, c, :], in_=K.xT[:, c, :])),
                                 reads=[K.XT[c, tc] for tc in range(4)], dma_key="out"))
        fin = S.op("sp", None)
        fin.deps = [outs[-1]]
        sems = {e: es.enter_context(nc.semaphore("s_" + e)) for e in ENGS}
        dsem = {k: es.enter_context(nc.semaphore("d_" + k.replace("_", ""))) for k in dict.fromkeys(K.dma_keys)}
        run = S.emit(sems, dsem)
        with nc.Block() as block:
            @block.sync
            def _(e):
                run("sp", e)

            @block.tensor
            def _(e):
                run("pe", e)

            @block.scalar
            def _(e):
                run("act", e)

            @block.vector
            def _(e):
                run("dve", e)

            @block.gpsimd
            def _(e):
                run("pool", e)
    K.ninstr = {e: len(S.ops[e]) for e in ENGS}
    K.peak = K.A.peak
    return nc, K


def _rel_bucket(d):
    d = np.asarray(d)
    exact = 16
    d_f = np.maximum(d, 1).astype(np.float32)
    large = exact + (np.log(d_f / np.float32(exact)) / np.float32(math.log(128 / exact)) * np.float32(32 - exact)).astype(np.int32)
    large = np.minimum(large, 31)
    return np.where(d < exact, d, large)


def host_consts(inputs):
    bf = ml_dtypes.bfloat16
    c = {}
    s = np.arange(128)[:, None]
    t = np.arange(128)[None, :]
    c["c_trimask"] = (s < t).astype(np.float32)
    c["c_negmask"] = np.where(t > s, -BIG, 0.0).astype(np.float32)
    g = np.stack([inputs["norm_mix"][0], inputs["norm_ffn"][0], inputs["norm_mix"][1], inputs["norm_ffn"][1]])
    c["c_gains"] = np.ascontiguousarray(g.reshape(4, 8, 128).transpose(2, 0, 1).reshape(128, 32)).astype(np.float32)
    cw = inputs["ev_conv_w"][0]
    c["c_convw"] = np.ascontiguousarray(cw.reshape(3, 4, 128).transpose(2, 1, 0).reshape(128, 12)).astype(np.float32)
    c["c_qkg"] = np.stack([np.tile(inputs["od_q_gain"][0], 2), np.tile(inputs["od_k_gain"][0], 2)], axis=1).astype(np.float32)
    rb = inputs["rel_bias"]
    c["c_c31"] = np.ascontiguousarray(np.broadcast_to(rb[31][None, :], (128, 16))).astype(np.float32)
    c["c_eps"] = np.ascontiguousarray(np.broadcast_to(np.array([EPS, 64 * EPS], np.float32)[None, :], (128, 2)))
    c["c_one"] = np.ones((128, 1), np.float32)
    c["c_ident"] = np.eye(128, dtype=np.float32).astype(bf)
    c["c_ustrict"] = (s > t).astype(np.float32).astype(bf)
    c["c_ones"] = np.ones((128, 128), np.float32).astype(bf)
    blk = np.zeros((128, 128), np.float32)
    blk[:64, :64] = 1
    blk[64:, 64:] = 1
    c["c_blk"] = blk.astype(bf)
    c["c_ncT"] = (s > t).astype(np.float32).astype(bf)
    c["c_negI"] = (-BIG * np.eye(128, dtype=np.float32)).astype(bf)
    u = np.arange(256)[None, :]
    dist = np.maximum(u - s, 0)
    bidx = _rel_bucket(dist)
    tab = rb[bidx]
    c["biasT"] = np.ascontiguousarray(tab.transpose(0, 2, 1).reshape(128, 16 * 256)).astype(np.float32)
    return c


_CACHE = {}


def run_parts(parts, xT_list, inputs, consts):
    key = tuple(parts)
    if key not in _CACHE:
        _CACHE[key] = build(parts)
    nc, K = _CACHE[key]
    base = {k: np.ascontiguousarray(np.asarray(inputs[k])[0] if k in ("ev_w_in", "ev_w_out", "od_w_in", "od_w_out") else np.asarray(inputs[k]))
            for k in ("ev_w_in", "ev_w_out", "od_w_in", "od_w_out", "ffn_w_gate", "ffn_w_up", "ffn_w_down")}
    base.update(consts)
    in_maps = []
    for xT in xT_list:
        m = dict(base)
        m["xT"] = xT
        in_maps.append(m)
    res = run_bass_kernel_spmd(nc, in_maps, core_ids=list(range(len(xT_list))))
    return [r["yT"] for r in res.results]


PLAN = [("l0mix", "l0ffn", "l1mix", "l1ffn")]


def kernel(**inputs):
    inputs = {k: np.asarray(v) for k, v in inputs.items()}
    x = inputs["x"]
    consts = host_consts(inputs)
    xTs = [np.ascontiguousarray(x[b].T) for b in range(x.shape[0])]
    for parts in PLAN:
        xTs = run_parts(parts, xTs, inputs, consts)
    out = np.stack([np.ascontiguousarray(y.T) for y in xTs], axis=0)
    return out.astype(np.float32)
```

```python
import math
from contextlib import ExitStack

import numpy as np
import ml_dtypes
import concourse.bass as bass
import concourse.mybir as mybir
from concourse.bass_utils import run_bass_kernel_spmd

ACT = mybir.ActivationFunctionType
ALU = mybir.AluOpType
F32 = mybir.dt.float32
BF16 = mybir.dt.bfloat16
AX = mybir.AxisListType

S_LEN = 2048
D = 1024
FF = 2816
NFC = FF // 128
EPS = 1e-6
BIG = 32768.0
TOPK = 256
NBIS = 16
USE_FAST_RECIP = False
ENGS = ("pe", "act", "dve", "pool", "sp")


class Op:
    __slots__ = ("eng", "fn", "deps", "idx", "sig", "dma_key", "needed")

    def __init__(self, eng, fn):
        self.eng = eng
        self.fn = fn
        self.deps = []
        self.idx = -1
        self.sig = None
        self.dma_key = None
        self.needed = False


class T:
    __slots__ = ("name", "writer", "readers", "pending")

    def __init__(self, name, pending=()):
        self.name = name
        self.writer = None
        self.readers = []
        self.pending = list(pending)


def _compress(ops):
    last = {}
    out = []
    for o in ops:
        if o.dma_key is not None:
            out.append(o)
        elif o.eng not in last or last[o.eng].idx < o.idx:
            last[o.eng] = o
    out.extend(last.values())
    return out


class Region:
    def __init__(self, arena, name, off, size, pending):
        self.arena = arena
        self.name = name
        self.off = off
        self.size = size
        self.pending = pending
        self.tiles = []

    def f32(self):
        return self.arena.ap[:, self.off:self.off + self.size]

    def bf16(self):
        return self.arena.ap[:, self.off:self.off + self.size].bitcast(BF16)

    def tile(self, name=None):
        t = T(name or self.name, self.pending)
        self.tiles.append(t)
        return t

    def collect(self):
        ops = list(self.pending)
        for t in self.tiles:
            if t.writer is not None:
                ops.append(t.writer)
            ops.extend(t.readers)
        return _compress(ops)

    def retile(self, name=None):
        self.pending = self.collect()
        self.tiles = []
        return self.tile(name)


class Arena:
    def __init__(self, ap, words):
        self.ap = ap
        self.words = words
        self.live = []
        self.freed = []
        self.peak = 0

    def alloc(self, name, words):
        words = (words + 15) // 16 * 16
        self.live.sort(key=lambda r: r[0])
        pos = 0
        off = None
        for (o, s, _) in self.live:
            if o - pos >= words:
                off = pos
                break
            pos = o + s
        if off is None:
            if self.words - pos >= words:
                off = pos
            else:
                raise MemoryError(f"arena full allocating {name} ({words} words); live="
                                  f"{[(r.name, s) for (_, s, r) in self.live]}")
        pending = []
        for (o, s, ops) in self.freed:
            if o < off + words and off < o + s:
                pending.extend(ops)
        r = Region(self, name, off, words, _compress(pending))
        self.live.append((off, words, r))
        self.peak = max(self.peak, off + words)
        return r

    def free(self, region):
        self.live = [x for x in self.live if x[2] is not region]
        self.freed.append((region.off, region.size, region.collect()))


class Slot:
    def __init__(self, region, key):
        self.region = region
        self.key = key
        self.t = None


class SlotPool:
    def __init__(self, K, name, nslots, words, dma=False):
        self.K = K
        self.name = name
        self.slots = []
        for i in range(nslots):
            key = None
            if dma:
                key = f"{name}{i}"
                K.dma_keys.append(key)
            self.slots.append(Slot(K.A.alloc(f"{name}{i}", words), key))
        self.i = 0

    def next(self):
        s = self.slots[self.i % len(self.slots)]
        self.i += 1
        s.t = s.region.retile()
        return s

    def free(self):
        for s in self.slots:
            self.K.A.free(s.region)


class Sched:
    def __init__(self):
        self.ops = {e: [] for e in ENGS}
        self.dma_cnt = {}

    def op(self, eng, fn, reads=(), writes=(), dma_key=None):
        o = Op(eng, fn)
        o.idx = len(self.ops[eng])
        o.dma_key = dma_key
        deps = []
        for t in reads:
            if t.writer is not None:
                deps.append(t.writer)
        for t in writes:
            if t.writer is not None:
                deps.append(t.writer)
            deps.extend(t.readers)
            deps.extend(t.pending)
        for t in reads:
            t.readers.append(o)
        for t in writes:
            t.writer = o
            t.readers = []
        seen = set()
        for d in deps:
            if d is o or id(d) in seen:
                continue
            seen.add(id(d))
            if d.eng == "pe" and eng == "pe" and d.dma_key is None and dma_key is None:
                continue
            o.deps.append(d)
            d.needed = True
        self.ops[eng].append(o)
        return o

    def emit(self, sems, dma_sems):
        for e in ENGS:
            cnt = 0
            for o in self.ops[e]:
                if o.dma_key is not None:
                    c = self.dma_cnt.get(o.dma_key, 0) + 16
                    self.dma_cnt[o.dma_key] = c
                    o.sig = ("dma:" + o.dma_key, c)
                elif o.needed:
                    cnt += 1
                    o.sig = (e, cnt)

        def run(e, engobj):
            seen = {}
            for o in self.ops[e]:
                for d in o.deps:
                    sname, val = d.sig
                    if seen.get(sname, 0) >= val:
                        continue
                    seen[sname] = val
                    sem = dma_sems[sname[4:]] if sname.startswith("dma:") else sems[sname]
                    engobj.wait_ge(sem, val)
                if o.fn is None:
                    continue
                ins = o.fn(engobj)
                if o.sig is not None:
                    sname, val = o.sig
                    if sname.startswith("dma:"):
                        ins.then_inc(dma_sems[sname[4:]], 16)
                    else:
                        ins.then_inc(sems[sname], 1)
        return run


class K_:
    pass


def mm(K, out, lhsT, rhs, start, stop, reads, writes):
    return K.S.op("pe", lambda e: e.matmul(out, lhsT=lhsT, rhs=rhs, start=start, stop=stop),
                  reads=reads, writes=writes)


def act(K, out, in_, func, reads, writes, scale=1.0, bias=0.0):
    return K.S.op("act", lambda e: e.activation(out=out, in_=in_, func=func, bias=bias, scale=scale),
                  reads=reads, writes=writes)


def tt(K, out, in0, in1, op, reads, writes, eng="dve"):
    return K.S.op(eng, lambda e: e.tensor_tensor(out=out, in0=in0, in1=in1, op=op), reads=reads, writes=writes)


def ts(K, out, in0, s1, op0, reads, writes, s2=None, op1=None, accum=None):
    if op1 is None:
        return K.S.op("dve", lambda e: e.tensor_scalar(out=out, in0=in0, scalar1=s1, scalar2=None, op0=op0),
                      reads=reads, writes=writes)
    if accum is None:
        return K.S.op("dve", lambda e: e.tensor_scalar(out=out, in0=in0, scalar1=s1, scalar2=s2, op0=op0, op1=op1),
                      reads=reads, writes=writes)
    return K.S.op("dve", lambda e: e.tensor_scalar(out=out, in0=in0, scalar1=s1, scalar2=s2, op0=op0, op1=op1,
                                                    accum_out=accum), reads=reads, writes=writes)


def stt(K, out, in0, scalar, in1, op0, op1, reads, writes):
    return K.S.op("dve", lambda e: e.scalar_tensor_tensor(out=out, in0=in0, scalar=scalar, in1=in1, op0=op0, op1=op1),
                  reads=reads, writes=writes)


def wload(K, pool, views):
    s = pool.next()
    for dst, src in views:
        K.S.op("pool", (lambda e, d=dst(s), sr=src: e.dma_start(out=d, in_=sr)), writes=[s.t], dma_key=s.key)
    return s


def w3(slot, kc, cols):
    return slot.region.bf16()[:, :kc * cols].rearrange("p (c f) -> p c f", c=kc)


def wload_cols(K, pool, wview, c0, ncols, kc=8):
    s = wload(K, pool, [(lambda s_: w3(s_, kc, ncols), wview[:, :, c0:c0 + ncols])])
    return w3(s, kc, ncols), s.t


def rmsnorm(K, gidx, tcs, hT, HT, hcol0=0):
    sqp = SlotPool(K, "nsq", 2, 256)
    rsp = SlotPool(K, "nrs", 2, 512)
    for n, tc in enumerate(tcs):
        cs = slice(tc * 512, (tc + 1) * 512)
        hs = slice(tc * 512 - hcol0, (tc + 1) * 512 - hcol0)
        bank, bt = K.ps[6 + n % 2], K.PB[6 + n % 2]
        for c in range(8):
            sq = sqp.next()
            act(K, sq.region.bf16(), K.xT[:, c, cs], ACT.Square, [K.XT[c, tc]], [sq.t])
            mm(K, bank[:], K.c_ones, sq.region.bf16(), c == 0, c == 7, [sq.t, K.TC], [bt])
        rs = rsp.next()
        act(K, rs.region.f32(), bank[:], ACT.Ln, [bt], [rs.t], scale=1.0 / D, bias=K.c_eps[:, 0:1])
        act(K, rs.region.f32(), rs.region.f32(), ACT.Exp, [rs.t], [rs.t], scale=-0.5)
        for c in range(8):
            stt(K, hT[:, c, hs], K.xT[:, c, cs], K.gains[:, gidx, c:c + 1], rs.region.f32(), ALU.mult, ALU.mult,
                [K.XT[c, tc], rs.t, K.TC], [HT[c, tc]])
    sqp.free()
    rsp.free()


def l0_mixer(K):
    S, A = K.S, K.A
    wv = K.dram["ev_w_in"].rearrange("(c p) f -> p c f", p=128)
    wo = K.dram["ev_w_out"].rearrange("(c p) f -> p c f", p=128)
    r_h = A.alloc("hT", 8 * 1024)
    hT = r_h.bf16().rearrange("p (c t) -> p c t", c=8)
    HT = {(c, tc): r_h.tile() for c in range(8) for tc in range(4)}
    rmsnorm(K, 0, range(4), hT, HT)

    wpA = SlotPool(K, "wA", 4, 512, dma=True)
    r_cat = A.alloc("catT", 8 * 1024)
    catT = r_cat.bf16().rearrange("p (c t) -> p c t", c=8)
    CT = {(c, tc): r_cat.tile() for c in range(8) for tc in range(4)}

    r_v = A.alloc("v", 16 * 256)
    vsb = r_v.bf16().rearrange("p (i f) -> p i f", i=16)
    VT = [r_v.tile() for _ in range(16)]
    wpB = SlotPool(K, "wB", 1, 2048, dma=True)
    wvv, t_wvv = wload_cols(K, wpB, wv, 1024, 512)
    for i in range(16):
        bank, bt = K.ps[i % 2], K.PB[i % 2]
        for kc in range(8):
            mm(K, bank[:], hT[:, kc, i * 128:(i + 1) * 128], wvv[:, kc, :], kc == 0, kc == 7, [HT[kc, i // 4], t_wvv], [bt])
        act(K, vsb[:, i, :], bank[:], ACT.Copy, [bt], [VT[i]])
    wpB.free()

    qp = SlotPool(K, "qT", 2, 1024)
    kp = SlotPool(K, "kT", 2, 1024)
    ep = SlotPool(K, "sbe", 2, 512)
    spp = SlotPool(K, "sbsp", 4, 512)
    nlp = SlotPool(K, "sbnl", 3, 256)
    lsp = SlotPool(K, "sbls", 2, 256)
    tmpp = SlotPool(K, "sbtmp", 3, 512)
    wp = SlotPool(K, "sbw", 3, 256)
    PJ = {}

    def make_proj(hp, bank_q, bank_k, eng):
        wq, t_wq = wload_cols(K, wpA, wv, hp * 128, 128)
        wk, t_wk = wload_cols(K, wpA, wv, 512 + hp * 128, 128)
        qs, ks = qp.next(), kp.next()
        d = dict(qT=qs.region.bf16(), kT=ks.region.bf16(),
                 QT=[qs.region.tile() for _ in range(4)], KT=[ks.region.tile() for _ in range(4)])
        PJ[hp] = d
        units = []
        for tc in range(4):
            for (w_, t_w, dstT, TT, sc, bi) in ((wq, t_wq, d["qT"], d["QT"], 0.125, bank_q), (wk, t_wk, d["kT"], d["KT"], 1.0, bank_k)):
                def unit(tc=tc, w_=w_, t_w=t_w, dstT=dstT, TT=TT, sc=sc, bi=bi):
                    cs = slice(tc * 512, (tc + 1) * 512)
                    bank, bt = K.ps[bi], K.PB[bi]
                    for kc in range(8):
                        mm(K, bank[:], w_[:, kc, :], hT[:, kc, cs], kc == 0, kc == 7, [HT[kc, tc], t_w], [bt])
                    if eng == "act":
                        act(K, dstT[:, cs], bank[:], ACT.Copy, [bt], [TT[tc]], scale=sc)
                    else:
                        ts(K, dstT[:, cs], bank[:], sc, ALU.mult, [bt], [TT[tc]])
                units.append(unit)
        return units

    for u_ in make_proj(0, 0, 1, "act"):
        u_()
    for hp in range(4):
        qT, kT, QT, KT = PJ[hp]["qT"], PJ[hp]["kT"], PJ[hp]["QT"], PJ[hp]["KT"]
        nxt_units = make_proj(hp + 1, 7, 7, "dve") if hp < 3 else []
        tiles = []
        for hh in range(2):
            for qc in range(4):
                for b in range(4 * qc + 3, -1, -1):
                    tiles.append((hh, qc, b))
        st = {}

        def stage1(i):
            hh, qc, b = tiles[i]
            pb = hh * 64
            c0 = max(0, b - 4 * qc) * 128
            n = 512 - c0
            diag = b >= 4 * qc
            zb, zt = K.ps[i % 3], K.PB[i % 3]
            mm(K, zb[:, 0:n], kT[pb:pb + 64, b * 128:(b + 1) * 128], qT[pb:pb + 64, qc * 512 + c0:(qc + 1) * 512],
               True, True, [KT[b // 4], QT[qc]], [zt])
            e_, sp_ = ep.next(), spp.next()
            act(K, e_.region.f32()[:, 0:n], zb[:, 0:n], ACT.Exp, [zt], [e_.t], scale=-1.0)
            act(K, sp_.region.f32()[:, 0:n], e_.region.f32()[:, 0:n], ACT.Ln, [e_.t], [sp_.t], bias=K.c_one[:, 0:1])
            st[i] = dict(sp=sp_, n=n, c0=c0, diag=diag, zb=zb, zt=zt)

        def stage1b(i):
            d = st[i]
            n, zb, zt, sp_ = d["n"], d["zb"], d["zt"], d["sp"]
            nl_ = nlp.next()
            tt(K, nl_.region.bf16()[:, 0:n], zb[:, 0:n], sp_.region.f32()[:, 0:n], ALU.add, [zt, sp_.t], [nl_.t])
            if d["diag"]:
                tt(K, nl_.region.bf16()[:, 0:128], nl_.region.bf16()[:, 0:128], K.c_tri, ALU.mult, [nl_.t, K.TC], [nl_.t])
            d["nl"] = nl_

        def stage2(i):
            hh, qc, b = tiles[i]
            d = st[i]
            n, c0 = d["n"], d["c0"]
            first = b == 4 * qc + 3
            ab, at = K.ps[3 + i % 2], K.PB[3 + i % 2]
            nl = d["nl"].region.bf16()
            if first:
                la, lb = lsp.next(), lsp.next()
                S.op("pool", (lambda e, r=la: e.memset(r.region.bf16(), 0.0)), writes=[la.t])
                S.op("pool", (lambda e, r=lb: e.memset(r.region.bf16(), 0.0)), writes=[lb.t])
                st["ls"] = [la, lb]
                st["lsi"] = 0
            cur = st["ls"][st["lsi"] % 2]
            nxt = st["ls"][(st["lsi"] + 1) % 2]
            mm(K, ab[:, 0:n], K.c_ustrict, nl[:, 0:n], True, first, [d["nl"].t, K.TC], [at])
            if not first:
                mm(K, ab[:, 0:n], K.c_ones, cur.region.bf16()[:, c0:512], False, True, [cur.t, K.TC], [at])
            if b > 0:
                if c0 > 0:
                    pass
                tt(K, nxt.region.bf16()[:, c0:512], cur.region.bf16()[:, c0:512], nl[:, 0:n], ALU.add,
                   [cur.t, d["nl"].t], [nxt.t], eng="pool")
                st["lsi"] += 1
            tm = tmpp.next()
            tt(K, tm.region.f32()[:, 0:n], ab[:, 0:n], d["sp"].region.f32()[:, 0:n], ALU.add, [at, d["sp"].t], [tm.t])
            d["tm"] = tm

        def stage2b(i):
            d = st[i]
            n, tm = d["n"], d["tm"]
            w_ = wp.next()
            act(K, w_.region.bf16()[:, 0:n], tm.region.f32()[:, 0:n], ACT.Exp, [tm.t], [w_.t], scale=-1.0)
            if d["diag"]:
                tt(K, w_.region.bf16()[:, 0:128], w_.region.bf16()[:, 0:128], K.c_tri, ALU.mult, [w_.t, K.TC], [w_.t], eng="pool")
            d["w"] = w_

        def stage3(i):
            hh, qc, b = tiles[i]
            d = st[i]
            n, c0 = d["n"], d["c0"]
            pb = hh * 64
            h = hp * 2 + hh
            ob, ot = K.ps[5 + qc % 2], K.PB[5 + qc % 2]
            first = b == 4 * qc + 3
            w_ = d["w"].region.bf16()
            lhs = vsb[:, b, h * 64:(h + 1) * 64]
            pieces = [(0, n)] if not d["diag"] or n == 128 else [(0, 128), (128, n)]
            for pi, (a0, a1) in enumerate(pieces):
                mm(K, ob[pb:pb + 64, c0 + a0:c0 + a1], lhs, w_[:, a0:a1], first and pi == 0,
                   b == 0 and pi == len(pieces) - 1, [VT[b], d["w"].t], [ot])
            if b == 0:
                K.S.op("dve", (lambda e, o=ob, p=pb, q=qc, hp=hp: e.tensor_copy(out=catT[p:p + 64, hp, q * 512:(q + 1) * 512],
                                                                       in_=o[p:p + 64, :])),
                       reads=[ot], writes=[CT[hp, qc]])
            del st[i]

        N = len(tiles)
        for i in range(N + 4):
            if i < N:
                stage1(i)
            if 0 <= i - 1 < N:
                stage1b(i - 1)
            if 0 <= i - 2 < N:
                stage2(i - 2)
            if 0 <= i - 3 < N:
                stage2b(i - 3)
            if 0 <= i - 4 < N:
                stage3(i - 4)
            if nxt_units and i >= 30 and (i - 30) % 6 == 0:
                nxt_units.pop(0)()
        while nxt_units:
            nxt_units.pop(0)()
    for p_ in (qp, kp, ep, spp, nlp, lsp, tmpp, wp):
        p_.free()
    A.free(r_v)

    r_g = A.alloc("convg", 2064)
    g = r_g.f32()
    GT = [r_g.tile() for _ in range(5)]
    up_ = SlotPool(K, "cvu", 2, 512)
    y1p = SlotPool(K, "cvy", 2, 512)
    for cc in range(4):
        wb, t_wb = wload_cols(K, wpA, wv, 1536 + cc * 128, 128)
        wc, t_wc = wload_cols(K, wpA, wv, 2048 + cc * 128, 128)
        wu, t_wu = wload_cols(K, wpA, wv, 2560 + cc * 128, 128)
        S.op("dve", lambda e: e.memset(g[:, 0:16], 0.0), writes=[GT[4]])
        for tc in range(4):
            cs = slice(tc * 512, (tc + 1) * 512)
            banks = []
            for j, (w_, t_w) in enumerate(((wb, t_wb), (wc, t_wc), (wu, t_wu))):
                bi = (tc % 2) * 3 + j
                bank, bt = K.ps[bi], K.PB[bi]
                for kc in range(8):
                    mm(K, bank[:], w_[:, kc, :], hT[:, kc, cs], kc == 0, kc == 7, [HT[kc, tc], t_w], [bt])
                banks.append((bank, bt))
            u_ = up_.next()
            act(K, u_.region.f32(), banks[2][0][:], ACT.Copy, [banks[2][1]], [u_.t])
            tt(K, g[:, 16 + tc * 512:16 + (tc + 1) * 512], banks[1][0][:], u_.region.f32(), ALU.mult,
               [banks[1][1], u_.t], [GT[tc]])
            prev = GT[tc - 1] if tc > 0 else GT[4]
            y = y1p.next()
            gs = lambda sh: g[:, 16 + tc * 512 - sh:16 + (tc + 1) * 512 - sh]
            ts(K, y.region.f32(), gs(2), K.convw[:, cc, 0:1], ALU.mult, [GT[tc], prev, K.TC], [y.t])
            stt(K, y.region.f32(), gs(1), K.convw[:, cc, 1:2], y.region.f32(), ALU.mult, ALU.add, [GT[tc], prev, y.t, K.TC], [y.t])
            stt(K, y.region.f32(), gs(0), K.convw[:, cc, 2:3], y.region.f32(), ALU.mult, ALU.add, [GT[tc], y.t, K.TC], [y.t])
            tt(K, catT[:, 4 + cc, cs], banks[0][0][:], y.region.f32(), ALU.mult, [banks[0][1], y.t], [CT[4 + cc, tc]])
    up_.free()
    y1p.free()
    A.free(r_g)
    A.free(r_h)

    for dc in range(8):
        wob, t_wo = wload_cols(K, wpA, wo, dc * 128, 128)
        for tc in range(4):
            cs = slice(tc * 512, (tc + 1) * 512)
            bank, bt = K.ps[(dc * 4 + tc) % 4], K.PB[(dc * 4 + tc) % 4]
            for kc in range(8):
                mm(K, bank[:], wob[:, kc, :], catT[:, kc, cs], kc == 0, kc == 7, [CT[kc, tc], t_wo], [bt])
            tt(K, K.xT[:, dc, cs], bank[:], K.xT[:, dc, cs], ALU.add, [bt, K.XT[dc, tc]], [K.XT[dc, tc]])
    wpA.free()
    A.free(r_cat)


def ffn(K, layer, store_out=None):
    S, A = K.S, K.A
    wg = K.dram["ffn_w_gate"][layer].rearrange("(c p) f -> p c f", p=128)
    wu = K.dram["ffn_w_up"][layer].rearrange("(c p) f -> p c f", p=128)
    wd = K.dram["ffn_w_down"][layer].rearrange("(c p) f -> p c f", p=128)
    wpG = SlotPool(K, "wG", 4, 1024, dma=True)
    wpD = SlotPool(K, "wD", 2, 1408, dma=True)
    sgp = SlotPool(K, "sg", 2, 512)
    r_hs, hTs, HTs = [], [], []
    for half in range(2):
        r_h = A.alloc("hTf", 8 * 512)
        r_hs.append(r_h)
        hTs.append(r_h.bf16().rearrange("p (c t) -> p c t", c=8))
        HTs.append({(c, tc): r_h.tile() for c in range(8) for tc in (2 * half, 2 * half + 1)})
    rmsnorm(K, layer * 2 + 1, (0, 1), hTs[0], HTs[0], hcol0=0)
    for half in range(2):
        r_h, hT, HT = r_hs[half], hTs[half], HTs[half]
        r_g = A.alloc("gT", NFC * 512)
        gT = r_g.bf16().rearrange("p (c t) -> p c t", c=NFC)
        GT = {(fc, j): r_g.tile() for fc in range(NFC) for j in range(2)}
        n = 0
        for f2 in range(NFC // 2):
            wgb, t_wg = wload_cols(K, wpG, wg, f2 * 256, 256)
            wub, t_wu = wload_cols(K, wpG, wu, f2 * 256, 256)
            for fi in range(2):
                fc = f2 * 2 + fi
                for j in range(2):
                    tc = 2 * half + j
                    hs = slice(j * 512, (j + 1) * 512)
                    gb, gt_ = K.ps[(n % 2) * 2], K.PB[(n % 2) * 2]
                    ub, ut_ = K.ps[(n % 2) * 2 + 1], K.PB[(n % 2) * 2 + 1]
                    n += 1
                    for kc in range(8):
                        mm(K, gb[:], wgb[:, kc, fi * 128:(fi + 1) * 128], hT[:, kc, hs], kc == 0, kc == 7, [HT[kc, tc], t_wg], [gt_])
                    for kc in range(8):
                        mm(K, ub[:], wub[:, kc, fi * 128:(fi + 1) * 128], hT[:, kc, hs], kc == 0, kc == 7, [HT[kc, tc], t_wu], [ut_])
                    sg = sgp.next()
                    act(K, sg.region.f32(), gb[:], ACT.Silu, [gt_], [sg.t])
                    tt(K, gT[:, fc, hs], ub[:], sg.region.f32(), ALU.mult, [ut_, sg.t], [GT[fc, j]])
        A.free(r_h)
        if half == 0:
            rmsnorm(K, layer * 2 + 1, (2, 3), hTs[1], HTs[1], hcol0=1024)
        for dc in range(8):
            wdb, t_wd = wload_cols(K, wpD, wd, dc * 128, 128, kc=NFC)
            for j in range(2):
                tc = 2 * half + j
                hs = slice(j * 512, (j + 1) * 512)
                cs = slice(tc * 512, (tc + 1) * 512)
                bank, bt = K.ps[4 + (dc * 2 + j) % 4], K.PB[4 + (dc * 2 + j) % 4]
                for kc in range(NFC):
                    mm(K, bank[:], wdb[:, kc, :], gT[:, kc, hs], kc == 0, kc == NFC - 1, [GT[kc, j], t_wd], [bt])
                tt(K, K.xT[:, dc, cs], bank[:], K.xT[:, dc, cs], ALU.add, [bt, K.XT[dc, tc]], [K.XT[dc, tc]])
            if store_out is not None:
                hs2 = slice(half * 1024, (half + 1) * 1024)
                store_out.append(S.op("sp", (lambda e, dc=dc, hs2=hs2: e.dma_start(out=K.yv[:, dc, hs2], in_=K.xT[:, dc, hs2])),
                                      reads=[K.XT[dc, 2 * half], K.XT[dc, 2 * half + 1]], dma_key="out"))
        A.free(r_g)
    wpG.free()
    wpD.free()
    sgp.free()


QE = [0, 1, 2, 3, 8, 9, 10, 11]
QO = [4, 5, 6, 7, 12, 13, 14, 15]


def RECIP(e, out, in_):
    if USE_FAST_RECIP:
        return e.reciprocal_approx_fast(out=out, in_=in_)
    return e.reciprocal(out=out, in_=in_)


def l1_mixer(K):
    S, A = K.S, K.A
    wv = K.dram["od_w_in"].rearrange("(c p) f -> p c f", p=128)
    wo = K.dram["od_w_out"].rearrange("(c p) f -> p c f", p=128)
    r_h = A.alloc("hT1", 8 * 1024)
    hT = r_h.bf16().rearrange("p (c t) -> p c t", c=8)
    HT = {(c, tc): r_h.tile() for c in range(8) for tc in range(4)}
    rmsnorm(K, 2, range(4), hT, HT)

    wpA = SlotPool(K, "wA", 3, 512, dma=True)
    r_q = [A.alloc(f"q1_{tc}", 8 * 256) for tc in range(4)]
    qTc = [r.bf16().rearrange("p (c t) -> p c t", c=8) for r in r_q]
    QT = {(c, tc): r_q[tc].tile() for c in range(8) for tc in range(4)}
    r_k = A.alloc("k1", 2 * 1024)
    kT = r_k.bf16().rearrange("p (c t) -> p c t", c=2)
    KT = {(c, tc): r_k.tile() for c in range(2) for tc in range(4)}
    r_qi = [A.alloc(f"qi1_{tc}", 4 * 256) for tc in range(4)]
    qiTc = [r.bf16().rearrange("p (c t) -> p c t", c=4) for r in r_qi]
    QIT = {(c, tc): r_qi[tc].tile() for c in range(4) for tc in range(4)}
    r_ki = A.alloc("ki1", 1024)
    kiT = r_ki.bf16()
    KIT = [r_ki.tile() for _ in range(4)]
    r_v = A.alloc("v1", 16 * 128)
    vsb = r_v.bf16().rearrange("p (i f) -> p i f", i=16)
    VT = [r_v.tile() for _ in range(16)]
    r_wi = A.alloc("wi1", 128)
    wi = r_wi.f32().rearrange("p (i h) -> p i h", i=16)
    WIT = [r_wi.tile() for _ in range(16)]

    wpB = SlotPool(K, "wB", 1, 8 * 264 // 2, dma=True)
    sB = wload(K, wpB, [(lambda s_: w3(s_, 8, 264)[:, :, 0:256], wv[:, :, 1280:1536]),
                        (lambda s_: w3(s_, 8, 264)[:, :, 256:264], wv[:, :, 2112:2120])])
    wB = w3(sB, 8, 264)
    for i in range(16):
        bank, bt = K.ps[i % 2], K.PB[i % 2]
        for kc in range(8):
            mm(K, bank[:, 0:264], hT[:, kc, i * 128:(i + 1) * 128], wB[:, kc, :], kc == 0, kc == 7, [HT[kc, i // 4], sB.t], [bt])
        act(K, vsb[:, i, :], bank[:, 0:256], ACT.Copy, [bt], [VT[i]])
        K.S.op("dve", (lambda e, i=i, bank=bank: e.tensor_copy(out=wi[:, i, :], in_=bank[:, 256:264])), reads=[bt], writes=[WIT[i]])
    wpB.free()

    sqp = SlotPool(K, "qsq", 2, 256)
    rsp = SlotPool(K, "qrs", 2, 512)
    jobs = []
    for j in range(8):
        jobs.append(("q", j, [(0, QE[j] * 64), (64, QO[j] * 64)]))
    for j in range(2):
        jobs.append(("k", j, [(0, 1024 + j * 128), (64, 1024 + j * 128 + 64)]))
    for j in range(4):
        jobs.append(("qi", j, [(0, 1536 + j * 128), (64, 1536 + j * 128 + 64)]))
    jobs.append(("ki", 0, [(0, 2048), (64, 2048)]))
    nb = 0
    for kind, j, cols in jobs:
        s = wload(K, wpA, [((lambda s_, o=o: w3(s_, 8, 128)[:, :, o:o + 64]), wv[:, :, c:c + 64]) for (o, c) in cols])
        wblk = w3(s, 8, 128)
        for tc in range(4):
            cs = slice(tc * 512, (tc + 1) * 512)
            bank, bt = K.ps[nb % 3], K.PB[nb % 3]
            nb += 1
            for kc in range(8):
                mm(K, bank[:], wblk[:, kc, :], hT[:, kc, cs], kc == 0, kc == 7, [HT[kc, tc], s.t], [bt])
            if kind in ("q", "k"):
                sq = sqp.next()
                act(K, sq.region.bf16(), bank[:], ACT.Square, [bt], [sq.t])
                b2, bt2 = K.ps[3 + nb % 2], K.PB[3 + nb % 2]
                mm(K, b2[:], K.c_blk, sq.region.bf16(), True, True, [sq.t, K.TC], [bt2])
                rs = rsp.next()
                if kind == "q":
                    act(K, rs.region.f32(), b2[:], ACT.Ln, [bt2], [rs.t], scale=1.0, bias=K.c_eps[:, 1:2])
                else:
                    act(K, rs.region.f32(), b2[:], ACT.Ln, [bt2], [rs.t], scale=1.0 / 64, bias=K.c_eps[:, 0:1])
                act(K, rs.region.f32(), rs.region.f32(), ACT.Exp, [rs.t], [rs.t], scale=-0.5)
                if kind == "q":
                    stt(K, qTc[tc][:, j, :], bank[:], K.qkg[:, 0:1], rs.region.f32(), ALU.mult, ALU.mult, [bt, rs.t, K.TC], [QT[j, tc]])
                else:
                    stt(K, kT[:, j, cs], bank[:], K.qkg[:, 1:2], rs.region.f32(), ALU.mult, ALU.mult, [bt, rs.t, K.TC], [KT[j, tc]])
            elif kind == "qi":
                act(K, qiTc[tc][:, j, :], bank[:], ACT.Copy, [bt], [QIT[j, tc]])
            else:
                act(K, kiT[:, cs], bank[:], ACT.Copy, [bt], [KIT[tc]])
    sqp.free()
    rsp.free()
    A.free(r_h)

    r_db = A.alloc("dbT", 16 * 128)
    dbT = r_db.bf16().rearrange("p (h u) -> p h u", h=16)
    t_db = r_db.tile()
    r_bt = A.alloc("biasT", 16 * 256)
    t_bt = r_bt.tile()
    K.dma_keys.append("biasT")
    S.op("sp", lambda e: e.dma_start(out=r_bt.f32(), in_=K.dram["biasT"]), writes=[t_bt], dma_key="biasT")
    for h in range(16):
        ts(K, dbT[:, h, :], r_bt.f32()[:, h * 256:(h + 1) * 256], K.c31[:, h:h + 1], ALU.subtract, [t_bt, K.TC], [t_db])
    A.free(r_bt)

    FP8 = mybir.dt.float8e4
    r_nmt = [A.alloc("notMT0", 16 * 128), A.alloc("notMT1", 16 * 128)]
    nmtv = [r.f32().bitcast(FP8).rearrange("p (b t) -> p b t", b=16) for r in r_nmt]
    NMTS = {}

    def score_gen(qc):
        notMT = nmtv[qc % 2]
        NMT = [r_nmt[qc % 2].retile() if b == 0 else r_nmt[qc % 2].tile() for b in range(16)]
        NMTS[qc] = NMT
        if qc == 0:
            S.op("dve", lambda e: e.memset(notMT[:, 0:2, 0:256], 0.0), writes=NMT[0:2])
            for b in range(2):
                K.S.op("dve", (lambda e, b=b: e.tensor_copy(out=notMT[:, b, b * 128:(b + 1) * 128], in_=K.c_ncT, saturate=False)),
                       reads=[K.TC], writes=[NMT[b]])
        rp = SlotPool(K, "relu", 2, 512)
        smp = SlotPool(K, "bis", 4, 16)
        tmp_regions = []
        yield 1.0
        for tl in [[i for i in range(4 * qc, 4 * qc + 4) if i >= 2]]:
            info = {}
            chunks = []
            for i in tl:
                sc = Slot(A.alloc(f"sc{i}", 128 * (i + 1)), None)
                tmp_regions.append(sc.region)
                info[i] = dict(sc=sc, n=128 * (i + 1), tiles={})
                for k4 in range((128 * (i + 1) + 511) // 512):
                    ncols = min(512, 128 * (i + 1) - 512 * k4)
                    info[i]["tiles"][k4] = sc.region.tile()
                    chunks.append((i, k4, ncols))
            nb = 0
            for h in range(8):
                pb = (h % 2) * 64
                for (i, k4, ncols) in chunks:
                    sc = info[i]["sc"].region.f32()
                    stile = info[i]["tiles"][k4]
                    bank, bt = K.ps[6 + nb % 2], K.PB[6 + nb % 2]
                    nb += 1
                    mm(K, bank[:, 0:ncols], qiTc[i // 4][pb:pb + 64, h // 2, (i % 4) * 128:(i % 4 + 1) * 128],
                       kiT[pb:pb + 64, k4 * 512:k4 * 512 + ncols], True, True,
                       [QIT[h // 2, i // 4], KIT[k4]], [bt])
                    r_ = rp.next()
                    act(K, r_.region.f32()[:, 0:ncols], bank[:, 0:ncols], ACT.Relu, [bt], [r_.t])
                    dst = sc[:, k4 * 512:k4 * 512 + ncols]
                    if h == 0:
                        ts(K, dst, r_.region.f32()[:, 0:ncols], wi[:, i, 0:1], ALU.mult, [r_.t, WIT[i]], [stile])
                    else:
                        stt(K, dst, r_.region.f32()[:, 0:ncols], wi[:, i, h:h + 1], dst, ALU.mult, ALU.add,
                            [r_.t, WIT[i], stile], [stile])
                    yield 0.7
            bis = {}
            for i in tl:
                sm = smp.next()
                sc = info[i]["sc"].region.f32()
                n = info[i]["n"]
                allt = list(info[i]["tiles"].values())
                v = sm.region.f32()
                K.S.op("dve", (lambda e, v=v, sc=sc, n=n: e.tensor_reduce(out=v[:, 0:1], in_=sc[:, 0:n], axis=AX.X, op=ALU.max)),
                       reads=allt, writes=[sm.t])
                K.S.op("dve", (lambda e, v=v, sc=sc, n=n: e.tensor_reduce(out=v[:, 1:2], in_=sc[:, 0:n], axis=AX.X, op=ALU.min)),
                       reads=allt, writes=[sm.t])
                tt(K, v[:, 2:3], v[:, 0:1], v[:, 1:2], ALU.subtract, [sm.t], [sm.t])
                tt(K, sc[:, i * 128:(i + 1) * 128], sc[:, i * 128:(i + 1) * 128], K.c_negmask, ALU.add,
                   allt + [K.TC], allt)
                nm = Slot(A.alloc(f"nm{i}", 64 * (i + 1)), None)
                nm.t = nm.region.tile()
                tmp_regions.append(nm.region)
                if i % 2 == 0:
                    ts(K, v[:, 6:7], v[:, 1:2], -1.0, ALU.mult, [sm.t], [sm.t])
                bis[i] = (sm, v, sc, n, allt, nm)
                yield 1.0
            for it in range(1, NBIS + 1):
                f = 2.0 ** (-it)
                for i in tl:
                    sm, v, sc, n, allt, nm = bis[i]
                    if i % 2 == 0:
                        stt(K, v[:, 3:4], v[:, 2:3], -f, v[:, 6:7], ALU.mult, ALU.add, [sm.t], [sm.t])
                    else:
                        stt(K, v[:, 3:4], v[:, 2:3], f, v[:, 1:2], ALU.mult, ALU.add, [sm.t], [sm.t])
                for i in tl:
                    sm, v, sc, n, allt, nm = bis[i]
                    junk = nm.region.bf16()
                    if i % 2 == 0:
                        K.S.op("act", (lambda e, junk=junk, sc=sc, n=n, v=v: e.activation(
                            out=junk[:, 0:n], in_=sc[:, 0:n], func=ACT.Sign, bias=v[:, 3:4], scale=1.0, accum_out=v[:, 4:5])),
                            reads=allt + [sm.t], writes=[sm.t, nm.t])
                for i in tl:
                    sm, v, sc, n, allt, nm = bis[i]
                    junk = nm.region.bf16()
                    if i % 2 == 1:
                        ts(K, junk[:, 0:n], sc[:, 0:n], v[:, 3:4], ALU.is_ge, allt + [sm.t], [sm.t], s2=0.0, op1=ALU.add, accum=v[:, 4:5])
                for i in tl:
                    sm, v, sc, n, allt, nm = bis[i]
                    if i % 2 == 1:
                        ts(K, v[:, 5:6], v[:, 4:5], TOPK - 0.5, ALU.is_ge, [sm.t], [sm.t], s2=f, op1=ALU.mult)
                        stt(K, v[:, 1:2], v[:, 5:6], v[:, 2:3], v[:, 1:2], ALU.mult, ALU.add, [sm.t], [sm.t])
                for i in tl:
                    sm, v, sc, n, allt, nm = bis[i]
                    if i % 2 == 0:
                        ts(K, v[:, 5:6], v[:, 4:5], 2.0 * TOPK - 1.0 - n, ALU.is_ge, [sm.t], [sm.t], s2=-f, op1=ALU.mult)
                        stt(K, v[:, 6:7], v[:, 5:6], v[:, 2:3], v[:, 6:7], ALU.mult, ALU.add, [sm.t], [sm.t])
                yield 1.0 + sum(bis[i][3] for i in tl if i % 2 == 1) / 1000.0
            for i in tl:
                if i % 2 == 0:
                    ts(K, bis[i][1][:, 1:2], bis[i][1][:, 6:7], -1.0, ALU.mult, [bis[i][0].t], [bis[i][0].t])
            for i in tl:
                sm, v, sc, n, allt, nm = bis[i]
                ts(K, nm.region.bf16()[:, 0:n], sc[:, 0:n], v[:, 1:2], ALU.is_lt, allt + [sm.t], [nm.t])
            yield 1.5
            for b in range(tl[-1] + 1):
                ii = [i for i in tl if i >= b]
                bank, bt = K.ps[6 + b % 2], K.PB[6 + b % 2]
                bv = bank[:].bitcast(BF16)
                for x, i in enumerate(ii):
                    nm = bis[i][5]
                    K.S.op("pe", (lambda e, bv=bv, x=x, nm=nm, b=b: e.transpose(out=bv[:, x * 128:(x + 1) * 128],
                                                                              in_=nm.region.bf16()[:, b * 128:(b + 1) * 128],
                                                                              identity=K.c_ident)),
                           reads=[nm.t, K.TC], writes=[bt])
                c_lo = (ii[0] - 4 * qc) * 128
                K.S.op("act", (lambda e, o=notMT[:, b, c_lo:c_lo + 128 * len(ii)], i_=bv[:, 0:128 * len(ii)]:
                               e.activation(out=o, in_=i_, func=ACT.Copy, saturate=False)), reads=[bt], writes=[NMT[b]])
                yield 0.5
        for p_ in (rp, smp):
            p_.free()
        for r in tmp_regions:
            A.free(r)
        A.free(r_qi[qc])

    def score_units(qc):
        tl = [i for i in range(4 * qc, 4 * qc + 4) if i >= 2]
        nch = sum((128 * (i + 1) + 511) // 512 for i in tl)
        ndve = sum(128 * (i + 1) for i in tl if i % 2 == 1)
        return 1.0 + 8 * nch * 0.7 + len(tl) * 1.0 + NBIS * (1.0 + ndve / 1000.0) + 1.5 + (tl[-1] + 1) * 0.5

    def attn_gen(qc):
        notMT = nmtv[qc % 2]
        NMT = NMTS[qc]
        r_o = A.alloc("oT", 8 * 256)
        oT = r_o.bf16().rearrange("p (c t) -> p c t", c=8)
        OT = [r_o.tile() for _ in range(8)]
        pp = SlotPool(K, "pexp", 3, 256)
        rdp = SlotPool(K, "rden", 1, 512)
        tiles = []
        for j in range(8):
            for hh in range(2):
                for b in range(4 * qc + 4):
                    tiles.append((j, hh, b))
        st = {}

        def a1(i):
            j, hh, b = tiles[i]
            pb = hh * 64
            h = QE[j] if hh == 0 else QO[j]
            c0 = max(0, b - 4 * qc) * 128
            n = 512 - c0
            lb, lt = K.ps[i % 3], K.PB[i % 3]
            ulo = 512 * qc + c0 - 128 * b
            nbias = max(0, min(256 - ulo, n)) if ulo < 256 else 0
            mm(K, lb[:, 0:n], kT[pb:pb + 64, j // 4, b * 128:(b + 1) * 128], qTc[qc][pb:pb + 64, j, c0:512],
               True, False, [KT[j // 4, b // 4], QT[j, qc]], [lt])
            if nbias > 0:
                mm(K, lb[:, 0:nbias], K.c_ident, dbT[:, h, ulo:ulo + nbias], False, False, [t_db, K.TC], [lt])
            mm(K, lb[:, 0:n], K.c_negI, notMT[:, b, c0:512], False, True, [NMT[b], K.TC], [lt])
            st[i] = [None, n, c0, h, lb, lt]

        def a1b(i):
            _, n, c0, h, lb, lt = st[i]
            p_ = pp.next()
            act(K, p_.region.bf16()[:, 0:n], lb[:, 0:n], ACT.Exp, [lt, K.TC], [p_.t], bias=K.c31[:, h:h + 1])
            st[i][0] = p_

        def a2(i):
            j, hh, b = tiles[i]
            p_, n, c0, h, _lb, _lt = st.pop(i)
            g = h // 4
            hidx = (j * 2 + hh)
            ob, ot = K.ps[3 + hidx % 2], K.PB[3 + hidx % 2]
            last = b == 4 * qc + 3
            pr = p_.region.bf16()[:, 0:n]
            mm(K, ob[0:64, c0:512], vsb[:, b, g * 64:(g + 1) * 64], pr, b == 0, last, [VT[b], p_.t], [ot])
            mm(K, ob[64:128, c0:512], K.c_ones[:, 0:64], pr, b == 0, last, [p_.t, K.TC], [ot])
            if last:
                def fin(ob=ob, ot=ot, h=h):
                    rd = rdp.next()
                    act(K, rd.region.f32()[64:128, :], ob[64:128, :], ACT.Ln, [ot], [rd.t])
                    act(K, rd.region.f32()[64:128, :], rd.region.f32()[64:128, :], ACT.Exp, [rd.t], [rd.t], scale=-1.0)
                    oc, opb = h // 2, (h % 2) * 64
                    tt(K, oT[opb:opb + 64, oc, :], ob[0:64, :], rd.region.f32()[64:128, :], ALU.mult, [ot, rd.t], [OT[oc]])
                pend.append((i + 3, fin))

        N = len(tiles)
        pend = []
        for i in range(N + 6):
            if i < N:
                a1(i)
            if 0 <= i - 1 < N:
                a1b(i - 1)
            if 0 <= i - 2 < N:
                a2(i - 2)
            while pend and pend[0][0] <= i - 2:
                pend.pop(0)[1]()
            yield 0.7
        assert not pend
        pp.free()
        rdp.free()
        for dc in range(8):
            wob, t_wo = wload_cols(K, wpA, wo, dc * 128, 128)
            bank, bt = K.ps[5], K.PB[5]
            for kc in range(8):
                mm(K, bank[:], wob[:, kc, :], oT[:, kc, :], kc == 0, kc == 7, [OT[kc], t_wo], [bt])
            cs = slice(qc * 512, (qc + 1) * 512)
            tt(K, K.xT[:, dc, cs], bank[:], K.xT[:, dc, cs], ALU.add, [bt, K.XT[dc, qc]], [K.XT[dc, qc]])
            yield 1.0
        A.free(r_o)
        A.free(r_q[qc])

    def attn_units(qc):
        return (16 * (4 * qc + 4) + 6) * 0.7 + 8 * 1.0

    for _ in score_gen(0):
        pass
    for qc in range(4):
        ga, na = attn_gen(qc), attn_units(qc)
        gs, ns = (score_gen(qc + 1), score_units(qc + 1)) if qc < 3 else (None, 1)
        da = ds = 0.0
        a_alive, s_alive = True, gs is not None
        while a_alive or s_alive:
            if a_alive and (not s_alive or da * ns <= ds * na):
                try:
                    da += next(ga)
                except StopIteration:
                    a_alive = False
            elif s_alive:
                try:
                    ds += next(gs)
                except StopIteration:
                    s_alive = False
    wpA.free()
    for r in (r_k, r_ki, r_v, r_wi, r_db, r_nmt[0], r_nmt[1]):
        A.free(r)


CONST_F32 = {"trimask": (128, 128), "negmask": (128, 128), "gains": (128, 32), "convw": (128, 12),
             "qkg": (128, 2), "c31": (128, 16), "eps": (128, 2), "one": (128, 1)}
CONST_BF = {"ident": (128, 128), "ustrict": (128, 128), "ones": (128, 128), "blk": (128, 128),
            "ncT": (128, 128), "negI": (128, 128)}
ARENA_WORDS = 53200


def build(parts):
    nc = bass.Bass("TRN2", target_bir_lowering=False)
    K = K_()
    K.nc = nc
    dram = {}
    dram["xT"] = nc.dram_tensor("xT", [D, S_LEN], F32, kind="ExternalInput").ap()
    dram["yT"] = nc.dram_tensor("yT", [D, S_LEN], F32, kind="ExternalOutput").ap()
    shapes = {"ev_w_in": [D, 3072], "ev_w_out": [D, D], "od_w_in": [D, 2120], "od_w_out": [D, D],
              "ffn_w_gate": [2, D, FF], "ffn_w_up": [2, D, FF], "ffn_w_down": [2, FF, D], "biasT": [128, 16 * 256]}
    for k, shp in shapes.items():
        dram[k] = nc.dram_tensor(k, shp, F32, kind="ExternalInput").ap()
    for k, shp in CONST_F32.items():
        dram["c_" + k] = nc.dram_tensor("c_" + k, list(shp), F32, kind="ExternalInput").ap()
    for k, shp in CONST_BF.items():
        dram["c_" + k] = nc.dram_tensor("c_" + k, list(shp), BF16, kind="ExternalInput").ap()
    K.dram = dram
    K.S = Sched()
    K.dma_keys = []
    with ExitStack() as es:
        arena_t = es.enter_context(nc.sbuf_tensor("arena", [128, ARENA_WORDS], F32))
        K.ps = [es.enter_context(nc.psum_tensor(f"ps{i}", [128, 512], F32)) for i in range(8)]
        K.PB = [T(f"pb{i}") for i in range(8)]
        K.A = Arena(arena_t[:], ARENA_WORDS)
        A, S = K.A, K.S
        K.TC = T("consts")
        cw = sum(s[1] for s in CONST_F32.values()) + sum(s[1] // 2 for s in CONST_BF.values())
        r_c = A.alloc("consts", cw)
        off = 0
        cap = {}
        for k, shp in CONST_F32.items():
            ap = r_c.f32()[:, off:off + shp[1]]
            key = "c_" + k
            K.dma_keys.append(key)
            S.op("sp", (lambda e, ap=ap, key=key: e.dma_start(out=ap, in_=dram[key])), writes=[K.TC], dma_key=key)
            cap[k] = ap
            off += shp[1]
        for k, shp in CONST_BF.items():
            ap = r_c.f32()[:, off:off + shp[1] // 2].bitcast(BF16)
            key = "c_" + k
            K.dma_keys.append(key)
            S.op("sp", (lambda e, ap=ap, key=key: e.dma_start(out=ap, in_=dram[key])), writes=[K.TC], dma_key=key)
            cap[k] = ap
            off += shp[1] // 2
        K.c_tri, K.c_negmask = cap["trimask"], cap["negmask"]
        K.gains = cap["gains"].rearrange("p (n c) -> p n c", n=4)
        K.convw = cap["convw"].rearrange("p (c w) -> p c w", c=4)
        K.qkg, K.c31, K.c_eps, K.c_one = cap["qkg"], cap["c31"], cap["eps"], cap["one"]
        K.c_ident, K.c_ustrict, K.c_ones, K.c_blk = cap["ident"], cap["ustrict"], cap["ones"], cap["blk"]
        K.c_ncT, K.c_negI = cap["ncT"], cap["negI"]
        r_x = A.alloc("xT", 8 * S_LEN)
        K.xT = r_x.f32().rearrange("p (c t) -> p c t", c=8)
        K.XT = {(c, tc): r_x.tile() for c in range(8) for tc in range(4)}
        xv = dram["xT"].rearrange("(c p) t -> p c t", p=128)
        for tc in range(4):
            key = f"x{tc}"
            K.dma_keys.append(key)
            S.op("sp", (lambda e, tc=tc: e.dma_start(out=K.xT[:, :, tc * 512:(tc + 1) * 512], in_=xv[:, :, tc * 512:(tc + 1) * 512])),
                 writes=[K.XT[c, tc] for c in range(8)], dma_key=key)
        if "l0mix" in parts:
            l0_mixer(K)
        if "l0ffn" in parts:
            ffn(K, 0)
        if "l1mix" in parts:
            l1_mixer(K)
        K.yv = dram["yT"].rearrange("(c p) t -> p c t", p=128)
        K.dma_keys.append("out")
        outs = []
        if "l1ffn" in parts:
            ffn(K, 1, store_out=outs)
        else:
            for c in range(8):
                outs.append(S.op("sp", (lambda e, c=c: e.dma_start(out=K.yv[:, c, :], in_=K.xT[:, c, :])),
                                 reads=[K.XT[c, tc] for tc in range(4)], dma_key="out"))
        fin = S.op("sp", None)
        fin.deps = [outs[-1]]
        sems = {e: es.enter_context(nc.semaphore("s_" + e)) for e in ENGS}
        dsem = {k: es.enter_context(nc.semaphore("d_" + k.replace("_", ""))) for k in dict.fromkeys(K.dma_keys)}
        run = S.emit(sems, dsem)
        with nc.Block() as block:
            @block.sync
            def _(e):
                run("sp", e)

            @block.tensor
            def _(e):
                run("pe", e)

            @block.scalar
            def _(e):
                run("act", e)

            @block.vector
            def _(e):
                run("dve", e)

            @block.gpsimd
            def _(e):
                run("pool", e)
    K.ninstr = {e: len(S.ops[e]) for e in ENGS}
    K.peak = K.A.peak
    return nc, K


def _rel_bucket(d):
    d = np.asarray(d)
    exact = 16
    d_f = np.maximum(d, 1).astype(np.float32)
    large = exact + (np.log(d_f / np.float32(exact)) / np.float32(math.log(128 / exact)) * np.float32(32 - exact)).astype(np.int32)
    large = np.minimum(large, 31)
    return np.where(d < exact, d, large)


def host_consts(inputs):
    bf = ml_dtypes.bfloat16
    c = {}
    s = np.arange(128)[:, None]
    t = np.arange(128)[None, :]
    c["c_trimask"] = (s < t).astype(np.float32)
    c["c_negmask"] = np.where(t > s, -BIG, 0.0).astype(np.float32)
    g = np.stack([inputs["norm_mix"][0], inputs["norm_ffn"][0], inputs["norm_mix"][1], inputs["norm_ffn"][1]])
    c["c_gains"] = np.ascontiguousarray(g.reshape(4, 8, 128).transpose(2, 0, 1).reshape(128, 32)).astype(np.float32)
    cw = inputs["ev_conv_w"][0]
    c["c_convw"] = np.ascontiguousarray(cw.reshape(3, 4, 128).transpose(2, 1, 0).reshape(128, 12)).astype(np.float32)
    c["c_qkg"] = np.stack([np.tile(inputs["od_q_gain"][0], 2), np.tile(inputs["od_k_gain"][0], 2)], axis=1).astype(np.float32)
    rb = inputs["rel_bias"]
    c["c_c31"] = np.ascontiguousarray(np.broadcast_to(rb[31][None, :], (128, 16))).astype(np.float32)
    c["c_eps"] = np.ascontiguousarray(np.broadcast_to(np.array([EPS, 64 * EPS], np.float32)[None, :], (128, 2)))
    c["c_one"] = np.ones((128, 1), np.float32)
    c["c_ident"] = np.eye(128, dtype=np.float32).astype(bf)
    c["c_ustrict"] = (s > t).astype(np.float32).astype(bf)
    c["c_ones"] = np.ones((128, 128), np.float32).astype(bf)
    blk = np.zeros((128, 128), np.float32)
    blk[:64, :64] = 1
    blk[64:, 64:] = 1
    c["c_blk"] = blk.astype(bf)
    c["c_ncT"] = (s > t).astype(np.float32).astype(bf)
    c["c_negI"] = (-BIG * np.eye(128, dtype=np.float32)).astype(bf)
    u = np.arange(256)[None, :]
    dist = np.maximum(u - s, 0)
    bidx = _rel_bucket(dist)
    tab = rb[bidx]
    c["biasT"] = np.ascontiguousarray(tab.transpose(0, 2, 1).reshape(128, 16 * 256)).astype(np.float32)
    return c


_CACHE = {}


def run_parts(parts, xT_list, inputs, consts):
    key = tuple(parts)
    if key not in _CACHE:
        _CACHE[key] = build(parts)
    nc, K = _CACHE[key]
    base = {k: np.ascontiguousarray(np.asarray(inputs[k])[0] if k in ("ev_w_in", "ev_w_out", "od_w_in", "od_w_out") else np.asarray(inputs[k]))
            for k in ("ev_w_in", "ev_w_out", "od_w_in", "od_w_out", "ffn_w_gate", "ffn_w_up", "ffn_w_down")}
    base.update(consts)
    in_maps = []
    for xT in xT_list:
        m = dict(base)
        m["xT"] = xT
        in_maps.append(m)
    res = run_bass_kernel_spmd(nc, in_maps, core_ids=list(range(len(xT_list))))
    return [r["yT"] for r in res.results]


PLAN = [("l0mix", "l0ffn", "l1mix", "l1ffn")]


def kernel(**inputs):
    inputs = {k: np.asarray(v) for k, v in inputs.items()}
    x = inputs["x"]
    consts = host_consts(inputs)
    xTs = [np.ascontiguousarray(x[b].T) for b in range(x.shape[0])]
    for parts in PLAN:
        xTs = run_parts(parts, xTs, inputs, consts)
    out = np.stack([np.ascontiguousarray(y.T) for y in xTs], axis=0)
    return out.astype(np.float32)
```

```python
import math
from contextlib import ExitStack

import numpy as np
import ml_dtypes
import concourse.bass as bass
import concourse.mybir as mybir
from concourse.bass_utils import run_bass_kernel_spmd

ACT = mybir.ActivationFunctionType
ALU = mybir.AluOpType
F32 = mybir.dt.float32
BF16 = mybir.dt.bfloat16
AX = mybir.AxisListType

S_LEN = 2048
D = 1024
FF = 2816
NFC = FF // 128
EPS = 1e-6
BIG = 32768.0
TOPK = 256
NBIS = 16
USE_FAST_RECIP = False
ENGS = ("pe", "act", "dve", "pool", "sp")


class Op:
    __slots__ = ("eng", "fn", "deps", "idx", "sig", "dma_key", "needed")

    def __init__(self, eng, fn):
        self.eng = eng
        self.fn = fn
        self.deps = []
        self.idx = -1
        self.sig = None
        self.dma_key = None
        self.needed = False


class T:
    __slots__ = ("name", "writer", "readers", "pending")

    def __init__(self, name, pending=()):
        self.name = name
        self.writer = None
        self.readers = []
        self.pending = list(pending)


def _compress(ops):
    last = {}
    out = []
    for o in ops:
        if o.dma_key is not None:
            out.append(o)
        elif o.eng not in last or last[o.eng].idx < o.idx:
            last[o.eng] = o
    out.extend(last.values())
    return out


class Region:
    def __init__(self, arena, name, off, size, pending):
        self.arena = arena
        self.name = name
        self.off = off
        self.size = size
        self.pending = pending
        self.tiles = []

    def f32(self):
        return self.arena.ap[:, self.off:self.off + self.size]

    def bf16(self):
        return self.arena.ap[:, self.off:self.off + self.size].bitcast(BF16)

    def tile(self, name=None):
        t = T(name or self.name, self.pending)
        self.tiles.append(t)
        return t

    def collect(self):
        ops = list(self.pending)
        for t in self.tiles:
            if t.writer is not None:
                ops.append(t.writer)
            ops.extend(t.readers)
        return _compress(ops)

    def retile(self, name=None):
        self.pending = self.collect()
        self.tiles = []
        return self.tile(name)


class Arena:
    def __init__(self, ap, words):
        self.ap = ap
        self.words = words
        self.live = []
        self.freed = []
        self.peak = 0

    def alloc(self, name, words):
        words = (words + 15) // 16 * 16
        self.live.sort(key=lambda r: r[0])
        pos = 0
        off = None
        for (o, s, _) in self.live:
            if o - pos >= words:
                off = pos
                break
            pos = o + s
        if off is None:
            if self.words - pos >= words:
                off = pos
            else:
                raise MemoryError(f"arena full allocating {name} ({words} words); live="
                                  f"{[(r.name, s) for (_, s, r) in self.live]}")
        pending = []
        for (o, s, ops) in self.freed:
            if o < off + words and off < o + s:
                pending.extend(ops)
        r = Region(self, name, off, words, _compress(pending))
        self.live.append((off, words, r))
        self.peak = max(self.peak, off + words)
        return r

    def free(self, region):
        self.live = [x for x in self.live if x[2] is not region]
        self.freed.append((region.off, region.size, region.collect()))


class Slot:
    def __init__(self, region, key):
        self.region = region
        self.key = key
        self.t = None


class SlotPool:
    def __init__(self, K, name, nslots, words, dma=False):
        self.K = K
        self.name = name
        self.slots = []
        for i in range(nslots):
            key = None
            if dma:
                key = f"{name}{i}"
                K.dma_keys.append(key)
            self.slots.append(Slot(K.A.alloc(f"{name}{i}", words), key))
        self.i = 0

    def next(self):
        s = self.slots[self.i % len(self.slots)]
        self.i += 1
        s.t = s.region.retile()
        return s

    def free(self):
        for s in self.slots:
            self.K.A.free(s.region)


class Sched:
    def __init__(self):
        self.ops = {e: [] for e in ENGS}
        self.dma_cnt = {}

    def op(self, eng, fn, reads=(), writes=(), dma_key=None):
        o = Op(eng, fn)
        o.idx = len(self.ops[eng])
        o.dma_key = dma_key
        deps = []
        for t in reads:
            if t.writer is not None:
                deps.append(t.writer)
        for t in writes:
            if t.writer is not None:
                deps.append(t.writer)
            deps.extend(t.readers)
            deps.extend(t.pending)
        for t in reads:
            t.readers.append(o)
        for t in writes:
            t.writer = o
            t.readers = []
        seen = set()
        for d in deps:
            if d is o or id(d) in seen:
                continue
            seen.add(id(d))
            if d.eng == "pe" and eng == "pe" and d.dma_key is None and dma_key is None:
                continue
            o.deps.append(d)
            d.needed = True
        self.ops[eng].append(o)
        return o

    def emit(self, sems, dma_sems):
        for e in ENGS:
            cnt = 0
            for o in self.ops[e]:
                if o.dma_key is not None:
                    c = self.dma_cnt.get(o.dma_key, 0) + 16
                    self.dma_cnt[o.dma_key] = c
                    o.sig = ("dma:" + o.dma_key, c)
                elif o.needed:
                    cnt += 1
                    o.sig = (e, cnt)

        def run(e, engobj):
            seen = {}
            for o in self.ops[e]:
                for d in o.deps:
                    sname, val = d.sig
                    if seen.get(sname, 0) >= val:
                        continue
                    seen[sname] = val
                    sem = dma_sems[sname[4:]] if sname.startswith("dma:") else sems[sname]
                    engobj.wait_ge(sem, val)
                if o.fn is None:
                    continue
                ins = o.fn(engobj)
                if o.sig is not None:
                    sname, val = o.sig
                    if sname.startswith("dma:"):
                        ins.then_inc(dma_sems[sname[4:]], 16)
                    else:
                        ins.then_inc(sems[sname], 1)
        return run


class K_:
    pass


def mm(K, out, lhsT, rhs, start, stop, reads, writes):
    return K.S.op("pe", lambda e: e.matmul(out, lhsT=lhsT, rhs=rhs, start=start, stop=stop),
                  reads=reads, writes=writes)


def act(K, out, in_, func, reads, writes, scale=1.0, bias=0.0):
    return K.S.op("act", lambda e: e.activation(out=out, in_=in_, func=func, bias=bias, scale=scale),
                  reads=reads, writes=writes)


def tt(K, out, in0, in1, op, reads, writes, eng="dve"):
    return K.S.op(eng, lambda e: e.tensor_tensor(out=out, in0=in0, in1=in1, op=op), reads=reads, writes=writes)


def ts(K, out, in0, s1, op0, reads, writes, s2=None, op1=None, accum=None):
    if op1 is None:
        return K.S.op("dve", lambda e: e.tensor_scalar(out=out, in0=in0, scalar1=s1, scalar2=None, op0=op0),
                      reads=reads, writes=writes)
    if accum is None:
        return K.S.op("dve", lambda e: e.tensor_scalar(out=out, in0=in0, scalar1=s1, scalar2=s2, op0=op0, op1=op1),
                      reads=reads, writes=writes)
    return K.S.op("dve", lambda e: e.tensor_scalar(out=out, in0=in0, scalar1=s1, scalar2=s2, op0=op0, op1=op1,
                                                    accum_out=accum), reads=reads, writes=writes)


def stt(K, out, in0, scalar, in1, op0, op1, reads, writes):
    return K.S.op("dve", lambda e: e.scalar_tensor_tensor(out=out, in0=in0, scalar=scalar, in1=in1, op0=op0, op1=op1),
                  reads=reads, writes=writes)


def wload(K, pool, views):
    s = pool.next()
    for dst, src in views:
        K.S.op("pool", (lambda e, d=dst(s), sr=src: e.dma_start(out=d, in_=sr)), writes=[s.t], dma_key=s.key)
    return s


def w3(slot, kc, cols):
    return slot.region.bf16()[:, :kc * cols].rearrange("p (c f) -> p c f", c=kc)


def wload_cols(K, pool, wview, c0, ncols, kc=8):
    s = wload(K, pool, [(lambda s_: w3(s_, kc, ncols), wview[:, :, c0:c0 + ncols])])
    return w3(s, kc, ncols), s.t


def rmsnorm(K, gidx, tcs, hT, HT, hcol0=0):
    sqp = SlotPool(K, "nsq", 2, 256)
    rsp = SlotPool(K, "nrs", 2, 512)
    for n, tc in enumerate(tcs):
        cs = slice(tc * 512, (tc + 1) * 512)
        hs = slice(tc * 512 - hcol0, (tc + 1) * 512 - hcol0)
        bank, bt = K.ps[6 + n % 2], K.PB[6 + n % 2]
        for c in range(8):
            sq = sqp.next()
            act(K, sq.region.bf16(), K.xT[:, c, cs], ACT.Square, [K.XT[c, tc]], [sq.t])
            mm(K, bank[:], K.c_ones, sq.region.bf16(), c == 0, c == 7, [sq.t, K.TC], [bt])
        rs = rsp.next()
        act(K, rs.region.f32(), bank[:], ACT.Ln, [bt], [rs.t], scale=1.0 / D, bias=K.c_eps[:, 0:1])
        act(K, rs.region.f32(), rs.region.f32(), ACT.Exp, [rs.t], [rs.t], scale=-0.5)
        for c in range(8):
            stt(K, hT[:, c, hs], K.xT[:, c, cs], K.gains[:, gidx, c:c + 1], rs.region.f32(), ALU.mult, ALU.mult,
                [K.XT[c, tc], rs.t, K.TC], [HT[c, tc]])
    sqp.free()
    rsp.free()


def l0_mixer(K):
    S, A = K.S, K.A
    wv = K.dram["ev_w_in"].rearrange("(c p) f -> p c f", p=128)
    wo = K.dram["ev_w_out"].rearrange("(c p) f -> p c f", p=128)
    r_h = A.alloc("hT", 8 * 1024)
    hT = r_h.bf16().rearrange("p (c t) -> p c t", c=8)
    HT = {(c, tc): r_h.tile() for c in range(8) for tc in range(4)}
    rmsnorm(K, 0, range(4), hT, HT)

    wpA = SlotPool(K, "wA", 4, 512, dma=True)
    r_cat = A.alloc("catT", 8 * 1024)
    catT = r_cat.bf16().rearrange("p (c t) -> p c t", c=8)
    CT = {(c, tc): r_cat.tile() for c in range(8) for tc in range(4)}

    r_v = A.alloc("v", 16 * 256)
    vsb = r_v.bf16().rearrange("p (i f) -> p i f", i=16)
    VT = [r_v.tile() for _ in range(16)]
    wpB = SlotPool(K, "wB", 1, 2048, dma=True)
    wvv, t_wvv = wload_cols(K, wpB, wv, 1024, 512)
    for i in range(16):
        bank, bt = K.ps[i % 2], K.PB[i % 2]
        for kc in range(8):
            mm(K, bank[:], hT[:, kc, i * 128:(i + 1) * 128], wvv[:, kc, :], kc == 0, kc == 7, [HT[kc, i // 4], t_wvv], [bt])
        act(K, vsb[:, i, :], bank[:], ACT.Copy, [bt], [VT[i]])
    wpB.free()

    qp = SlotPool(K, "qT", 2, 1024)
    kp = SlotPool(K, "kT", 2, 1024)
    ep = SlotPool(K, "sbe", 2, 512)
    spp = SlotPool(K, "sbsp", 4, 512)
    nlp = SlotPool(K, "sbnl", 3, 256)
    lsp = SlotPool(K, "sbls", 2, 256)
    tmpp = SlotPool(K, "sbtmp", 3, 512)
    wp = SlotPool(K, "sbw", 3, 256)
    PJ = {}

    def make_proj(hp, bank_q, bank_k, eng):
        wq, t_wq = wload_cols(K, wpA, wv, hp * 128, 128)
        wk, t_wk = wload_cols(K, wpA, wv, 512 + hp * 128, 128)
        qs, ks = qp.next(), kp.next()
        d = dict(qT=qs.region.bf16(), kT=ks.region.bf16(),
                 QT=[qs.region.tile() for _ in range(4)], KT=[ks.region.tile() for _ in range(4)])
        PJ[hp] = d
        units = []
        for tc in range(4):
            for (w_, t_w, dstT, TT, sc, bi) in ((wq, t_wq, d["qT"], d["QT"], 0.125, bank_q), (wk, t_wk, d["kT"], d["KT"], 1.0, bank_k)):
                def unit(tc=tc, w_=w_, t_w=t_w, dstT=dstT, TT=TT, sc=sc, bi=bi):
                    cs = slice(tc * 512, (tc + 1) * 512)
                    bank, bt = K.ps[bi], K.PB[bi]
                    for kc in range(8):
                        mm(K, bank[:], w_[:, kc, :], hT[:, kc, cs], kc == 0, kc == 7, [HT[kc, tc], t_w], [bt])
                    if eng == "act":
                        act(K, dstT[:, cs], bank[:], ACT.Copy, [bt], [TT[tc]], scale=sc)
                    else:
                        ts(K, dstT[:, cs], bank[:], sc, ALU.mult, [bt], [TT[tc]])
                units.append(unit)
        return units

    for u_ in make_proj(0, 0, 1, "act"):
        u_()
    for hp in range(4):
        qT, kT, QT, KT = PJ[hp]["qT"], PJ[hp]["kT"], PJ[hp]["QT"], PJ[hp]["KT"]
        nxt_units = make_proj(hp + 1, 7, 7, "dve") if hp < 3 else []
        tiles = []
        for hh in range(2):
            for qc in range(4):
                for b in range(4 * qc + 3, -1, -1):
                    tiles.append((hh, qc, b))
        st = {}

        def stage1(i):
            hh, qc, b = tiles[i]
            pb = hh * 64
            c0 = max(0, b - 4 * qc) * 128
            n = 512 - c0
            diag = b >= 4 * qc
            zb, zt = K.ps[i % 3], K.PB[i % 3]
            mm(K, zb[:, 0:n], kT[pb:pb + 64, b * 128:(b + 1) * 128], qT[pb:pb + 64, qc * 512 + c0:(qc + 1) * 512],
               True, True, [KT[b // 4], QT[qc]], [zt])
            e_, sp_ = ep.next(), spp.next()
            act(K, e_.region.f32()[:, 0:n], zb[:, 0:n], ACT.Exp, [zt], [e_.t], scale=-1.0)
            act(K, sp_.region.f32()[:, 0:n], e_.region.f32()[:, 0:n], ACT.Ln, [e_.t], [sp_.t], bias=K.c_one[:, 0:1])
            st[i] = dict(sp=sp_, n=n, c0=c0, diag=diag, zb=zb, zt=zt)

        def stage1b(i):
            d = st[i]
            n, zb, zt, sp_ = d["n"], d["zb"], d["zt"], d["sp"]
            nl_ = nlp.next()
            tt(K, nl_.region.bf16()[:, 0:n], zb[:, 0:n], sp_.region.f32()[:, 0:n], ALU.add, [zt, sp_.t], [nl_.t])
            if d["diag"]:
                tt(K, nl_.region.bf16()[:, 0:128], nl_.region.bf16()[:, 0:128], K.c_tri, ALU.mult, [nl_.t, K.TC], [nl_.t])
            d["nl"] = nl_

        def stage2(i):
            hh, qc, b = tiles[i]
            d = st[i]
            n, c0 = d["n"], d["c0"]
            first = b == 4 * qc + 3
            ab, at = K.ps[3 + i % 2], K.PB[3 + i % 2]
            nl = d["nl"].region.bf16()
            if first:
                la, lb = lsp.next(), lsp.next()
                S.op("pool", (lambda e, r=la: e.memset(r.region.bf16(), 0.0)), writes=[la.t])
                S.op("pool", (lambda e, r=lb: e.memset(r.region.bf16(), 0.0)), writes=[lb.t])
                st["ls"] = [la, lb]
                st["lsi"] = 0
            cur = st["ls"][st["lsi"] % 2]
            nxt = st["ls"][(st["lsi"] + 1) % 2]
            mm(K, ab[:, 0:n], K.c_ustrict, nl[:, 0:n], True, first, [d["nl"].t, K.TC], [at])
            if not first:
                mm(K, ab[:, 0:n], K.c_ones, cur.region.bf16()[:, c0:512], False, True, [cur.t, K.TC], [at])
            if b > 0:
                if c0 > 0:
                    pass
                tt(K, nxt.region.bf16()[:, c0:512], cur.region.bf16()[:, c0:512], nl[:, 0:n], ALU.add,
                   [cur.t, d["nl"].t], [nxt.t], eng="pool")
                st["lsi"] += 1
            tm = tmpp.next()
            tt(K, tm.region.f32()[:, 0:n], ab[:, 0:n], d["sp"].region.f32()[:, 0:n], ALU.add, [at, d["sp"].t], [tm.t])
            d["tm"] = tm

        def stage2b(i):
            d = st[i]
            n, tm = d["n"], d["tm"]
            w_ = wp.next()
            act(K, w_.region.bf16()[:, 0:n], tm.region.f32()[:, 0:n], ACT.Exp, [tm.t], [w_.t], scale=-1.0)
            if d["diag"]:
                tt(K, w_.region.bf16()[:, 0:128], w_.region.bf16()[:, 0:128], K.c_tri, ALU.mult, [w_.t, K.TC], [w_.t], eng="pool")
            d["w"] = w_

        def stage3(i):
            hh, qc, b = tiles[i]
            d = st[i]
            n, c0 = d["n"], d["c0"]
            pb = hh * 64
            h = hp * 2 + hh
            ob, ot = K.ps[5 + qc % 2], K.PB[5 + qc % 2]
            first = b == 4 * qc + 3
            w_ = d["w"].region.bf16()
            lhs = vsb[:, b, h * 64:(h + 1) * 64]
            pieces = [(0, n)] if not d["diag"] or n == 128 else [(0, 128), (128, n)]
            for pi, (a0, a1) in enumerate(pieces):
                mm(K, ob[pb:pb + 64, c0 + a0:c0 + a1], lhs, w_[:, a0:a1], first and pi == 0,
                   b == 0 and pi == len(pieces) - 1, [VT[b], d["w"].t], [ot])
            if b == 0:
                K.S.op("dve", (lambda e, o=ob, p=pb, q=qc, hp=hp: e.tensor_copy(out=catT[p:p + 64, hp, q * 512:(q + 1) * 512],
                                                                       in_=o[p:p + 64, :])),
                       reads=[ot], writes=[CT[hp, qc]])
            del st[i]

        N = len(tiles)
        for i in range(N + 4):
            if i < N:
                stage1(i)
            if 0 <= i - 1 < N:
                stage1b(i - 1)
            if 0 <= i - 2 < N:
                stage2(i - 2)
            if 0 <= i - 3 < N:
                stage2b(i - 3)
            if 0 <= i - 4 < N:
                stage3(i - 4)
            if nxt_units and i >= 30 and (i - 30) % 6 == 0:
                nxt_units.pop(0)()
        while nxt_units:
            nxt_units.pop(0)()
    for p_ in (qp, kp, ep, spp, nlp, lsp, tmpp, wp):
        p_.free()
    A.free(r_v)

    r_g = A.alloc("convg", 2064)
    g = r_g.f32()
    GT = [r_g.tile() for _ in range(5)]
    up_ = SlotPool(K, "cvu", 2, 512)
    y1p = SlotPool(K, "cvy", 2, 512)
    for cc in range(4):
        wb, t_wb = wload_cols(K, wpA, wv, 1536 + cc * 128, 128)
        wc, t_wc = wload_cols(K, wpA, wv, 2048 + cc * 128, 128)
        wu, t_wu = wload_cols(K, wpA, wv, 2560 + cc * 128, 128)
        S.op("dve", lambda e: e.memset(g[:, 0:16], 0.0), writes=[GT[4]])
        for tc in range(4):
            cs = slice(tc * 512, (tc + 1) * 512)
            banks = []
            for j, (w_, t_w) in enumerate(((wb, t_wb), (wc, t_wc), (wu, t_wu))):
                bi = (tc % 2) * 3 + j
                bank, bt = K.ps[bi], K.PB[bi]
                for kc in range(8):
                    mm(K, bank[:], w_[:, kc, :], hT[:, kc, cs], kc == 0, kc == 7, [HT[kc, tc], t_w], [bt])
                banks.append((bank, bt))
            u_ = up_.next()
            act(K, u_.region.f32(), banks[2][0][:], ACT.Copy, [banks[2][1]], [u_.t])
            tt(K, g[:, 16 + tc * 512:16 + (tc + 1) * 512], banks[1][0][:], u_.region.f32(), ALU.mult,
               [banks[1][1], u_.t], [GT[tc]])
            prev = GT[tc - 1] if tc > 0 else GT[4]
            y = y1p.next()
            gs = lambda sh: g[:, 16 + tc * 512 - sh:16 + (tc + 1) * 512 - sh]
            ts(K, y.region.f32(), gs(2), K.convw[:, cc, 0:1], ALU.mult, [GT[tc], prev, K.TC], [y.t])
            stt(K, y.region.f32(), gs(1), K.convw[:, cc, 1:2], y.region.f32(), ALU.mult, ALU.add, [GT[tc], prev, y.t, K.TC], [y.t])
            stt(K, y.region.f32(), gs(0), K.convw[:, cc, 2:3], y.region.f32(), ALU.mult, ALU.add, [GT[tc], y.t, K.TC], [y.t])
            tt(K, catT[:, 4 + cc, cs], banks[0][0][:], y.region.f32(), ALU.mult, [banks[0][1], y.t], [CT[4 + cc, tc]])
    up_.free()
    y1p.free()
    A.free(r_g)
    A.free(r_h)

    for dc in range(8):
        wob, t_wo = wload_cols(K, wpA, wo, dc * 128, 128)
        for tc in range(4):
            cs = slice(tc * 512, (tc + 1) * 512)
            bank, bt = K.ps[(dc * 4 + tc) % 4], K.PB[(dc * 4 + tc) % 4]
            for kc in range(8):
                mm(K, bank[:], wob[:, kc, :], catT[:, kc, cs], kc == 0, kc == 7, [CT[kc, tc], t_wo], [bt])
            tt(K, K.xT[:, dc, cs], bank[:], K.xT[:, dc, cs], ALU.add, [bt, K.XT[dc, tc]], [K.XT[dc, tc]])
    wpA.free()
    A.free(r_cat)


def ffn(K, layer, store_out=None):
    S, A = K.S, K.A
    wg = K.dram["ffn_w_gate"][layer].rearrange("(c p) f -> p c f", p=128)
    wu = K.dram["ffn_w_up"][layer].rearrange("(c p) f -> p c f", p=128)
    wd = K.dram["ffn_w_down"][layer].rearrange("(c p) f -> p c f", p=128)
    wpG = SlotPool(K, "wG", 4, 1024, dma=True)
    wpD = SlotPool(K, "wD", 2, 1408, dma=True)
    sgp = SlotPool(K, "sg", 2, 512)
    r_hs, hTs, HTs = [], [], []
    for half in range(2):
        r_h = A.alloc("hTf", 8 * 512)
        r_hs.append(r_h)
        hTs.append(r_h.bf16().rearrange("p (c t) -> p c t", c=8))
        HTs.append({(c, tc): r_h.tile() for c in range(8) for tc in (2 * half, 2 * half + 1)})
    rmsnorm(K, layer * 2 + 1, (0, 1), hTs[0], HTs[0], hcol0=0)
    for half in range(2):
        r_h, hT, HT = r_hs[half], hTs[half], HTs[half]
        r_g = A.alloc("gT", NFC * 512)
        gT = r_g.bf16().rearrange("p (c t) -> p c t", c=NFC)
        GT = {(fc, j): r_g.tile() for fc in range(NFC) for j in range(2)}
        n = 0
        for f2 in range(NFC // 2):
            wgb, t_wg = wload_cols(K, wpG, wg, f2 * 256, 256)
            wub, t_wu = wload_cols(K, wpG, wu, f2 * 256, 256)
            for fi in range(2):
                fc = f2 * 2 + fi
                for j in range(2):
                    tc = 2 * half + j
                    hs = slice(j * 512, (j + 1) * 512)
                    gb, gt_ = K.ps[(n % 2) * 2], K.PB[(n % 2) * 2]
                    ub, ut_ = K.ps[(n % 2) * 2 + 1], K.PB[(n % 2) * 2 + 1]
                    n += 1
                    for kc in range(8):
                        mm(K, gb[:], wgb[:, kc, fi * 128:(fi + 1) * 128], hT[:, kc, hs], kc == 0, kc == 7, [HT[kc, tc], t_wg], [gt_])
                    for kc in range(8):
                        mm(K, ub[:], wub[:, kc, fi * 128:(fi + 1) * 128], hT[:, kc, hs], kc == 0, kc == 7, [HT[kc, tc], t_wu], [ut_])
                    sg = sgp.next()
                    act(K, sg.region.f32(), gb[:], ACT.Silu, [gt_], [sg.t])
                    tt(K, gT[:, fc, hs], ub[:], sg.region.f32(), ALU.mult, [ut_, sg.t], [GT[fc, j]])
        A.free(r_h)
        if half == 0:
            rmsnorm(K, layer * 2 + 1, (2, 3), hTs[1], HTs[1], hcol0=1024)
        for dc in range(8):
            wdb, t_wd = wload_cols(K, wpD, wd, dc * 128, 128, kc=NFC)
            for j in range(2):
                tc = 2 * half + j
                hs = slice(j * 512, (j + 1) * 512)
                cs = slice(tc * 512, (tc + 1) * 512)
                bank, bt = K.ps[4 + (dc * 2 + j) % 4], K.PB[4 + (dc * 2 + j) % 4]
                for kc in range(NFC):
                    mm(K, bank[:], wdb[:, kc, :], gT[:, kc, hs], kc == 0, kc == NFC - 1, [GT[kc, j], t_wd], [bt])
                tt(K, K.xT[:, dc, cs], bank[:], K.xT[:, dc, cs], ALU.add, [bt, K.XT[dc, tc]], [K.XT[dc, tc]])
            if store_out is not None:
                hs2 = slice(half * 1024, (half + 1) * 1024)
                store_out.append(S.op("sp", (lambda e, dc=dc, hs2=hs2: e.dma_start(out=K.yv[:, dc, hs2], in_=K.xT[:, dc, hs2])),
                                      reads=[K.XT[dc, 2 * half], K.XT[dc, 2 * half + 1]], dma_key="out"))
        A.free(r_g)
    wpG.free()
    wpD.free()
    sgp.free()


QE = [0, 1, 2, 3, 8, 9, 10, 11]
QO = [4, 5, 6, 7, 12, 13, 14, 15]


def RECIP(e, out, in_):
    if USE_FAST_RECIP:
        return e.reciprocal_approx_fast(out=out, in_=in_)
    return e.reciprocal(out=out, in_=in_)


def l1_mixer(K):
    S, A = K.S, K.A
    wv = K.dram["od_w_in"].rearrange("(c p) f -> p c f", p=128)
    wo = K.dram["od_w_out"].rearrange("(c p) f -> p c f", p=128)
    r_h = A.alloc("hT1", 8 * 1024)
    hT = r_h.bf16().rearrange("p (c t) -> p c t", c=8)
    HT = {(c, tc): r_h.tile() for c in range(8) for tc in range(4)}
    rmsnorm(K, 2, range(4), hT, HT)

    wpA = SlotPool(K, "wA", 2, 512, dma=True)
    r_q = [A.alloc(f"q1_{tc}", 8 * 256) for tc in range(4)]
    qTc = [r.bf16().rearrange("p (c t) -> p c t", c=8) for r in r_q]
    QT = {(c, tc): r_q[tc].tile() for c in range(8) for tc in range(4)}
    r_k = A.alloc("k1", 2 * 1024)
    kT = r_k.bf16().rearrange("p (c t) -> p c t", c=2)
    KT = {(c, tc): r_k.tile() for c in range(2) for tc in range(4)}
    r_qi = [A.alloc(f"qi1_{tc}", 4 * 256) for tc in range(4)]
    qiTc = [r.bf16().rearrange("p (c t) -> p c t", c=4) for r in r_qi]
    QIT = {(c, tc): r_qi[tc].tile() for c in range(4) for tc in range(4)}
    r_ki = A.alloc("ki1", 1024)
    kiT = r_ki.bf16()
    KIT = [r_ki.tile() for _ in range(4)]
    r_v = A.alloc("v1", 16 * 256)
    vext = r_v.bf16().rearrange("p (i g f) -> p i g f", i=16, g=4)
    VT = [r_v.tile() for _ in range(16)]
    r_wi = A.alloc("wi1", 128)
    wi = r_wi.f32().rearrange("p (i h) -> p i h", i=16)
    WIT = [r_wi.tile() for _ in range(16)]

    wpB = SlotPool(K, "wB", 1, 8 * 264 // 2, dma=True)
    sB = wload(K, wpB, [(lambda s_: w3(s_, 8, 264)[:, :, 0:256], wv[:, :, 1280:1536]),
                        (lambda s_: w3(s_, 8, 264)[:, :, 256:264], wv[:, :, 2112:2120])])
    wB = w3(sB, 8, 264)
    S.op("dve", lambda e: e.memset(vext[:, :, :, 64:128], 1.0), writes=VT)
    for i in range(16):
        bank, bt = K.ps[i % 2], K.PB[i % 2]
        for kc in range(8):
            mm(K, bank[:, 0:264], hT[:, kc, i * 128:(i + 1) * 128], wB[:, kc, :], kc == 0, kc == 7, [HT[kc, i // 4], sB.t], [bt])
        act(K, vext[:, i, :, 0:64], bank[:, 0:256].rearrange("p (g f) -> p g f", g=4), ACT.Copy, [bt], [VT[i]])
        K.S.op("dve", (lambda e, i=i, bank=bank: e.tensor_copy(out=wi[:, i, :], in_=bank[:, 256:264])), reads=[bt], writes=[WIT[i]])
    wpB.free()

    sqp = SlotPool(K, "qsq", 2, 256)
    rsp = SlotPool(K, "qrs", 2, 512)
    jobs = []
    for j in range(8):
        jobs.append(("q", j, [(0, QE[j] * 64), (64, QO[j] * 64)]))
    for j in range(2):
        jobs.append(("k", j, [(0, 1024 + j * 128), (64, 1024 + j * 128 + 64)]))
    for j in range(4):
        jobs.append(("qi", j, [(0, 1536 + j * 128), (64, 1536 + j * 128 + 64)]))
    jobs.append(("ki", 0, [(0, 2048), (64, 2048)]))
    nb = 0
    for kind, j, cols in jobs:
        s = wload(K, wpA, [((lambda s_, o=o: w3(s_, 8, 128)[:, :, o:o + 64]), wv[:, :, c:c + 64]) for (o, c) in cols])
        wblk = w3(s, 8, 128)
        for tc in range(4):
            cs = slice(tc * 512, (tc + 1) * 512)
            bank, bt = K.ps[nb % 3], K.PB[nb % 3]
            nb += 1
            for kc in range(8):
                mm(K, bank[:], wblk[:, kc, :], hT[:, kc, cs], kc == 0, kc == 7, [HT[kc, tc], s.t], [bt])
            if kind in ("q", "k"):
                sq = sqp.next()
                act(K, sq.region.bf16(), bank[:], ACT.Square, [bt], [sq.t])
                b2, bt2 = K.ps[3 + nb % 2], K.PB[3 + nb % 2]
                mm(K, b2[:], K.c_blk, sq.region.bf16(), True, True, [sq.t, K.TC], [bt2])
                rs = rsp.next()
                if kind == "q":
                    act(K, rs.region.f32(), b2[:], ACT.Ln, [bt2], [rs.t], scale=1.0, bias=K.c_eps[:, 1:2])
                else:
                    act(K, rs.region.f32(), b2[:], ACT.Ln, [bt2], [rs.t], scale=1.0 / 64, bias=K.c_eps[:, 0:1])
                act(K, rs.region.f32(), rs.region.f32(), ACT.Exp, [rs.t], [rs.t], scale=-0.5)
                if kind == "q":
                    stt(K, qTc[tc][:, j, :], bank[:], K.qkg[:, 0:1], rs.region.f32(), ALU.mult, ALU.mult, [bt, rs.t, K.TC], [QT[j, tc]])
                else:
                    stt(K, kT[:, j, cs], bank[:], K.qkg[:, 1:2], rs.region.f32(), ALU.mult, ALU.mult, [bt, rs.t, K.TC], [KT[j, tc]])
            elif kind == "qi":
                act(K, qiTc[tc][:, j, :], bank[:], ACT.Copy, [bt], [QIT[j, tc]])
            else:
                act(K, kiT[:, cs], bank[:], ACT.Copy, [bt], [KIT[tc]])
    sqp.free()
    rsp.free()
    A.free(r_h)

    r_db = A.alloc("dbT", 16 * 128)
    dbT = r_db.bf16().rearrange("p (h u) -> p h u", h=16)
    t_db = r_db.tile()
    r_bt = A.alloc("biasT", 16 * 256)
    t_bt = r_bt.tile()
    K.dma_keys.append("biasT")
    S.op("sp", lambda e: e.dma_start(out=r_bt.f32(), in_=K.dram["biasT"]), writes=[t_bt], dma_key="biasT")
    for h in range(16):
        ts(K, dbT[:, h, :], r_bt.f32()[:, h * 256:(h + 1) * 256], K.c31[:, h:h + 1], ALU.subtract, [t_bt, K.TC], [t_db])
    A.free(r_bt)

    FP8 = mybir.dt.float8e4
    r_nmt = [A.alloc("notMT0", 16 * 128), A.alloc("notMT1", 16 * 128)]
    nmtv = [r.f32().bitcast(FP8).rearrange("p (b t) -> p b t", b=16) for r in r_nmt]
    NMTS = {}

    def score_gen(qc):
        notMT = nmtv[qc % 2]
        NMT = [r_nmt[qc % 2].retile() if b == 0 else r_nmt[qc % 2].tile() for b in range(16)]
        NMTS[qc] = NMT
        if qc == 0:
            S.op("dve", lambda e: e.memset(notMT[:, 0:2, 0:256], 0.0), writes=NMT[0:2])
            for b in range(2):
                K.S.op("dve", (lambda e, b=b: e.tensor_copy(out=notMT[:, b, b * 128:(b + 1) * 128], in_=K.c_ncT, saturate=False)),
                       reads=[K.TC], writes=[NMT[b]])
        rp = SlotPool(K, "relu", 2, 512)
        smp = SlotPool(K, "bis", 4, 16)
        tmp_regions = []
        yield 1.0
        for tl in [[i for i in range(4 * qc, 4 * qc + 4) if i >= 2]]:
            info = {}
            chunks = []
            for i in tl:
                sc = Slot(A.alloc(f"sc{i}", 128 * (i + 1)), None)
                tmp_regions.append(sc.region)
                info[i] = dict(sc=sc, n=128 * (i + 1), tiles={})
                for k4 in range((128 * (i + 1) + 511) // 512):
                    ncols = min(512, 128 * (i + 1) - 512 * k4)
                    info[i]["tiles"][k4] = sc.region.tile()
                    chunks.append((i, k4, ncols))
            nb = 0
            for h in range(8):
                pb = (h % 2) * 64
                for (i, k4, ncols) in chunks:
                    sc = info[i]["sc"].region.f32()
                    stile = info[i]["tiles"][k4]
                    bank, bt = K.ps[6 + nb % 2], K.PB[6 + nb % 2]
                    nb += 1
                    mm(K, bank[:, 0:ncols], qiTc[i // 4][pb:pb + 64, h // 2, (i % 4) * 128:(i % 4 + 1) * 128],
                       kiT[pb:pb + 64, k4 * 512:k4 * 512 + ncols], True, True,
                       [QIT[h // 2, i // 4], KIT[k4]], [bt])
                    r_ = rp.next()
                    act(K, r_.region.f32()[:, 0:ncols], bank[:, 0:ncols], ACT.Relu, [bt], [r_.t])
                    dst = sc[:, k4 * 512:k4 * 512 + ncols]
                    if h == 0:
                        ts(K, dst, r_.region.f32()[:, 0:ncols], wi[:, i, 0:1], ALU.mult, [r_.t, WIT[i]], [stile])
                    else:
                        stt(K, dst, r_.region.f32()[:, 0:ncols], wi[:, i, h:h + 1], dst, ALU.mult, ALU.add,
                            [r_.t, WIT[i], stile], [stile])
                    yield 0.7
            bis = {}
            for i in tl:
                sm = smp.next()
                sc = info[i]["sc"].region.f32()
                n = info[i]["n"]
                allt = list(info[i]["tiles"].values())
                v = sm.region.f32()
                K.S.op("dve", (lambda e, v=v, sc=sc, n=n: e.tensor_reduce(out=v[:, 0:1], in_=sc[:, 0:n], axis=AX.X, op=ALU.max)),
                       reads=allt, writes=[sm.t])
                K.S.op("dve", (lambda e, v=v, sc=sc, n=n: e.tensor_reduce(out=v[:, 1:2], in_=sc[:, 0:n], axis=AX.X, op=ALU.min)),
                       reads=allt, writes=[sm.t])
                tt(K, v[:, 2:3], v[:, 0:1], v[:, 1:2], ALU.subtract, [sm.t], [sm.t])
                tt(K, sc[:, i * 128:(i + 1) * 128], sc[:, i * 128:(i + 1) * 128], K.c_negmask, ALU.add,
                   allt + [K.TC], allt)
                nm = Slot(A.alloc(f"nm{i}", 64 * (i + 1)), None)
                nm.t = nm.region.tile()
                tmp_regions.append(nm.region)
                if i % 2 == 0:
                    ts(K, v[:, 6:7], v[:, 1:2], -1.0, ALU.mult, [sm.t], [sm.t])
                bis[i] = (sm, v, sc, n, allt, nm)
                yield 1.0
            for it in range(1, NBIS + 1):
                f = 2.0 ** (-it)
                for i in tl:
                    sm, v, sc, n, allt, nm = bis[i]
                    if i % 2 == 0:
                        stt(K, v[:, 3:4], v[:, 2:3], -f, v[:, 6:7], ALU.mult, ALU.add, [sm.t], [sm.t])
                    else:
                        stt(K, v[:, 3:4], v[:, 2:3], f, v[:, 1:2], ALU.mult, ALU.add, [sm.t], [sm.t])
                for i in tl:
                    sm, v, sc, n, allt, nm = bis[i]
                    junk = nm.region.bf16()
                    if i % 2 == 0:
                        K.S.op("act", (lambda e, junk=junk, sc=sc, n=n, v=v: e.activation(
                            out=junk[:, 0:n], in_=sc[:, 0:n], func=ACT.Sign, bias=v[:, 3:4], scale=1.0, accum_out=v[:, 4:5])),
                            reads=allt + [sm.t], writes=[sm.t, nm.t])
                for i in tl:
                    sm, v, sc, n, allt, nm = bis[i]
                    junk = nm.region.bf16()
                    if i % 2 == 1:
                        ts(K, junk[:, 0:n], sc[:, 0:n], v[:, 3:4], ALU.is_ge, allt + [sm.t], [sm.t], s2=0.0, op1=ALU.add, accum=v[:, 4:5])
                for i in tl:
                    sm, v, sc, n, allt, nm = bis[i]
                    if i % 2 == 1:
                        ts(K, v[:, 5:6], v[:, 4:5], TOPK - 0.5, ALU.is_ge, [sm.t], [sm.t], s2=f, op1=ALU.mult)
                        stt(K, v[:, 1:2], v[:, 5:6], v[:, 2:3], v[:, 1:2], ALU.mult, ALU.add, [sm.t], [sm.t])
                for i in tl:
                    sm, v, sc, n, allt, nm = bis[i]
                    if i % 2 == 0:
                        ts(K, v[:, 5:6], v[:, 4:5], 2.0 * TOPK - 1.0 - n, ALU.is_ge, [sm.t], [sm.t], s2=-f, op1=ALU.mult)
                        stt(K, v[:, 6:7], v[:, 5:6], v[:, 2:3], v[:, 6:7], ALU.mult, ALU.add, [sm.t], [sm.t])
                yield 1.0 + sum(bis[i][3] for i in tl if i % 2 == 1) / 1000.0
            for i in tl:
                if i % 2 == 0:
                    ts(K, bis[i][1][:, 1:2], bis[i][1][:, 6:7], -1.0, ALU.mult, [bis[i][0].t], [bis[i][0].t])
            for i in tl:
                sm, v, sc, n, allt, nm = bis[i]
                ts(K, nm.region.bf16()[:, 0:n], sc[:, 0:n], v[:, 1:2], ALU.is_lt, allt + [sm.t], [nm.t])
            yield 1.5
            for b in range(tl[-1] + 1):
                ii = [i for i in tl if i >= b]
                bank, bt = K.ps[6 + b % 2], K.PB[6 + b % 2]
                bv = bank[:].bitcast(BF16)
                for x, i in enumerate(ii):
                    nm = bis[i][5]
                    K.S.op("pe", (lambda e, bv=bv, x=x, nm=nm, b=b: e.transpose(out=bv[:, x * 128:(x + 1) * 128],
                                                                              in_=nm.region.bf16()[:, b * 128:(b + 1) * 128],
                                                                              identity=K.c_ident)),
                           reads=[nm.t, K.TC], writes=[bt])
                c_lo = (ii[0] - 4 * qc) * 128
                K.S.op("act", (lambda e, o=notMT[:, b, c_lo:c_lo + 128 * len(ii)], i_=bv[:, 0:128 * len(ii)]:
                               e.activation(out=o, in_=i_, func=ACT.Copy, saturate=False)), reads=[bt], writes=[NMT[b]])
                yield 0.5
        for p_ in (rp, smp):
            p_.free()
        for r in tmp_regions:
            A.free(r)
        A.free(r_qi[qc])

    def score_units(qc):
        tl = [i for i in range(4 * qc, 4 * qc + 4) if i >= 2]
        nch = sum((128 * (i + 1) + 511) // 512 for i in tl)
        ndve = sum(128 * (i + 1) for i in tl if i % 2 == 1)
        return 1.0 + 8 * nch * 0.7 + len(tl) * 1.0 + NBIS * (1.0 + ndve / 1000.0) + 1.5 + (tl[-1] + 1) * 0.5

    def attn_gen(qc):
        notMT = nmtv[qc % 2]
        NMT = NMTS[qc]
        r_o = A.alloc("oT", 8 * 256)
        oT = r_o.bf16().rearrange("p (c t) -> p c t", c=8)
        OT = [r_o.tile() for _ in range(8)]
        pp = SlotPool(K, "pexp", 3, 256)
        rdp = SlotPool(K, "rden", 1, 512)
        tiles = []
        for j in range(8):
            for hh in range(2):
                for b in range(4 * qc + 4):
                    tiles.append((j, hh, b))
        st = {}

        def a1(i):
            j, hh, b = tiles[i]
            pb = hh * 64
            h = QE[j] if hh == 0 else QO[j]
            c0 = max(0, b - 4 * qc) * 128
            n = 512 - c0
            lb, lt = K.ps[i % 3], K.PB[i % 3]
            ulo = 512 * qc + c0 - 128 * b
            nbias = max(0, min(256 - ulo, n)) if ulo < 256 else 0
            mm(K, lb[:, 0:n], kT[pb:pb + 64, j // 4, b * 128:(b + 1) * 128], qTc[qc][pb:pb + 64, j, c0:512],
               True, False, [KT[j // 4, b // 4], QT[j, qc]], [lt])
            if nbias > 0:
                mm(K, lb[:, 0:nbias], K.c_ident, dbT[:, h, ulo:ulo + nbias], False, False, [t_db, K.TC], [lt])
            mm(K, lb[:, 0:n], K.c_negI, notMT[:, b, c0:512], False, True, [NMT[b], K.TC], [lt])
            st[i] = [None, n, c0, h, lb, lt]

        def a1b(i):
            _, n, c0, h, lb, lt = st[i]
            p_ = pp.next()
            act(K, p_.region.bf16()[:, 0:n], lb[:, 0:n], ACT.Exp, [lt, K.TC], [p_.t], bias=K.c31[:, h:h + 1])
            st[i][0] = p_

        def a2(i):
            j, hh, b = tiles[i]
            p_, n, c0, h, _lb, _lt = st.pop(i)
            g = h // 4
            hidx = (j * 2 + hh)
            ob, ot = K.ps[3 + hidx % 2], K.PB[3 + hidx % 2]
            last = b == 4 * qc + 3
            pr = p_.region.bf16()[:, 0:n]
            mm(K, ob[:, c0:512], vext[:, b, g, :], pr, b == 0, last, [VT[b], p_.t], [ot])
            if last:
                def fin(ob=ob, ot=ot, h=h):
                    rd = rdp.next()
                    act(K, rd.region.f32()[64:128, :], ob[64:128, :], ACT.Ln, [ot], [rd.t])
                    act(K, rd.region.f32()[64:128, :], rd.region.f32()[64:128, :], ACT.Exp, [rd.t], [rd.t], scale=-1.0)
                    oc, opb = h // 2, (h % 2) * 64
                    tt(K, oT[opb:opb + 64, oc, :], ob[0:64, :], rd.region.f32()[64:128, :], ALU.mult, [ot, rd.t], [OT[oc]])
                pend.append((i + 3, fin))

        N = len(tiles)
        pend = []
        for i in range(N + 6):
            if i < N:
                a1(i)
            if 0 <= i - 1 < N:
                a1b(i - 1)
            if 0 <= i - 2 < N:
                a2(i - 2)
            while pend and pend[0][0] <= i - 2:
                pend.pop(0)[1]()
            yield 0.7
        assert not pend
        pp.free()
        rdp.free()
        for dc in range(8):
            wob, t_wo = wload_cols(K, wpA, wo, dc * 128, 128)
            bank, bt = K.ps[5], K.PB[5]
            for kc in range(8):
                mm(K, bank[:], wob[:, kc, :], oT[:, kc, :], kc == 0, kc == 7, [OT[kc], t_wo], [bt])
            cs = slice(qc * 512, (qc + 1) * 512)
            tt(K, K.xT[:, dc, cs], bank[:], K.xT[:, dc, cs], ALU.add, [bt, K.XT[dc, qc]], [K.XT[dc, qc]])
            yield 1.0
        A.free(r_o)
        A.free(r_q[qc])

    def attn_units(qc):
        return (16 * (4 * qc + 4) + 6) * 0.7 + 8 * 1.0

    for _ in score_gen(0):
        pass
    for qc in range(4):
        ga, na = attn_gen(qc), attn_units(qc)
        gs, ns = (score_gen(qc + 1), score_units(qc + 1)) if qc < 3 else (None, 1)
        da = ds = 0.0
        a_alive, s_alive = True, gs is not None
        while a_alive or s_alive:
            if a_alive and (not s_alive or da * ns <= ds * na):
                try:
                    da += next(ga)
                except StopIteration:
                    a_alive = False
            elif s_alive:
                try:
                    ds += next(gs)
                except StopIteration:
                    s_alive = False
    wpA.free()
    for r in (r_k, r_ki, r_v, r_wi, r_db, r_nmt[0], r_nmt[1]):
        A.free(r)


CONST_F32 = {"trimask": (128, 128), "negmask": (128, 128), "gains": (128, 32), "convw": (128, 12),
             "qkg": (128, 2), "c31": (128, 16), "eps": (128, 2), "one": (128, 1)}
CONST_BF = {"ident": (128, 128), "ustrict": (128, 128), "ones": (128, 128), "blk": (128, 128),
            "ncT": (128, 128), "negI": (128, 128)}
ARENA_WORDS = 53200


def build(parts):
    nc = bass.Bass("TRN2", target_bir_lowering=False)
    K = K_()
    K.nc = nc
    dram = {}
    dram["xT"] = nc.dram_tensor("xT", [D, S_LEN], F32, kind="ExternalInput").ap()
    dram["yT"] = nc.dram_tensor("yT", [D, S_LEN], F32, kind="ExternalOutput").ap()
    shapes = {"ev_w_in": [D, 3072], "ev_w_out": [D, D], "od_w_in": [D, 2120], "od_w_out": [D, D],
              "ffn_w_gate": [2, D, FF], "ffn_w_up": [2, D, FF], "ffn_w_down": [2, FF, D], "biasT": [128, 16 * 256]}
    for k, shp in shapes.items():
        dram[k] = nc.dram_tensor(k, shp, F32, kind="ExternalInput").ap()
    for k, shp in CONST_F32.items():
        dram["c_" + k] = nc.dram_tensor("c_" + k, list(shp), F32, kind="ExternalInput").ap()
    for k, shp in CONST_BF.items():
        dram["c_" + k] = nc.dram_tensor("c_" + k, list(shp), BF16, kind="ExternalInput").ap()
    K.dram = dram
    K.S = Sched()
    K.dma_keys = []
    with ExitStack() as es:
        arena_t = es.enter_context(nc.sbuf_tensor("arena", [128, ARENA_WORDS], F32))
        K.ps = [es.enter_context(nc.psum_tensor(f"ps{i}", [128, 512], F32)) for i in range(8)]
        K.PB = [T(f"pb{i}") for i in range(8)]
        K.A = Arena(arena_t[:], ARENA_WORDS)
        A, S = K.A, K.S
        K.TC = T("consts")
        cw = sum(s[1] for s in CONST_F32.values()) + sum(s[1] // 2 for s in CONST_BF.values())
        r_c = A.alloc("consts", cw)
        off = 0
        cap = {}
        for k, shp in CONST_F32.items():
            ap = r_c.f32()[:, off:off + shp[1]]
            key = "c_" + k
            K.dma_keys.append(key)
            S.op("sp", (lambda e, ap=ap, key=key: e.dma_start(out=ap, in_=dram[key])), writes=[K.TC], dma_key=key)
            cap[k] = ap
            off += shp[1]
        for k, shp in CONST_BF.items():
            ap = r_c.f32()[:, off:off + shp[1] // 2].bitcast(BF16)
            key = "c_" + k
            K.dma_keys.append(key)
            S.op("sp", (lambda e, ap=ap, key=key: e.dma_start(out=ap, in_=dram[key])), writes=[K.TC], dma_key=key)
            cap[k] = ap
            off += shp[1] // 2
        K.c_tri, K.c_negmask = cap["trimask"], cap["negmask"]
        K.gains = cap["gains"].rearrange("p (n c) -> p n c", n=4)
        K.convw = cap["convw"].rearrange("p (c w) -> p c w", c=4)
        K.qkg, K.c31, K.c_eps, K.c_one = cap["qkg"], cap["c31"], cap["eps"], cap["one"]
        K.c_ident, K.c_ustrict, K.c_ones, K.c_blk = cap["ident"], cap["ustrict"], cap["ones"], cap["blk"]
        K.c_ncT, K.c_negI = cap["ncT"], cap["negI"]
        r_x = A.alloc("xT", 8 * S_LEN)
        K.xT = r_x.f32().rearrange("p (c t) -> p c t", c=8)
        K.XT = {(c, tc): r_x.tile() for c in range(8) for tc in range(4)}
        xv = dram["xT"].rearrange("(c p) t -> p c t", p=128)
        for tc in range(4):
            key = f"x{tc}"
            K.dma_keys.append(key)
            S.op("sp", (lambda e, tc=tc: e.dma_start(out=K.xT[:, :, tc * 512:(tc + 1) * 512], in_=xv[:, :, tc * 512:(tc + 1) * 512])),
                 writes=[K.XT[c, tc] for c in range(8)], dma_key=key)
        if "l0mix" in parts:
            l0_mixer(K)
        if "l0ffn" in parts:
            ffn(K, 0)
        if "l1mix" in parts:
            l1_mixer(K)
        K.yv = dram["yT"].rearrange("(c p) t -> p c t", p=128)
        K.dma_keys.append("out")
        outs = []
        if "l1ffn" in parts:
            ffn(K, 1, store_out=outs)
        else:
            for c in range(8):
                outs.append(S.op("sp", (lambda e, c=c: e.dma_start(out=K.yv[:, c, :], in_=K.xT[:, c, :])),
                                 reads=[K.XT[c, tc] for tc in range(4)], dma_key="out"))
        fin = S.op("sp", None)
        fin.deps = [outs[-1]]
        sems = {e: es.enter_context(nc.semaphore("s_" + e)) for e in ENGS}
        dsem = {k: es.enter_context(nc.semaphore("d_" + k.replace("_", ""))) for k in dict.fromkeys(K.dma_keys)}
        run = S.emit(sems, dsem)
        with nc.Block() as block:
            @block.sync
            def _(e):
                run("sp", e)

            @block.tensor
            def _(e):
                run("pe", e)

            @block.scalar
            def _(e):
                run("act", e)

            @block.vector
            def _(e):
                run("dve", e)

            @block.gpsimd
            def _(e):
                run("pool", e)
    K.ninstr = {e: len(S.ops[e]) for e in ENGS}
    K.peak = K.A.peak
    return nc, K


def _rel_bucket(d):
    d = np.asarray(d)
    exact = 16
    d_f = np.maximum(d, 1).astype(np.float32)
    large = exact + (np.log(d_f / np.float32(exact)) / np.float32(math.log(128 / exact)) * np.float32(32 - exact)).astype(np.int32)
    large = np.minimum(large, 31)
    return np.where(d < exact, d, large)


def host_consts(inputs):
    bf = ml_dtypes.bfloat16
    c = {}
    s = np.arange(128)[:, None]
    t = np.arange(128)[None, :]
    c["c_trimask"] = (s < t).astype(np.float32)
    c["c_negmask"] = np.where(t > s, -BIG, 0.0).astype(np.float32)
    g = np.stack([inputs["norm_mix"][0], inputs["norm_ffn"][0], inputs["norm_mix"][1], inputs["norm_ffn"][1]])
    c["c_gains"] = np.ascontiguousarray(g.reshape(4, 8, 128).transpose(2, 0, 1).reshape(128, 32)).astype(np.float32)
    cw = inputs["ev_conv_w"][0]
    c["c_convw"] = np.ascontiguousarray(cw.reshape(3, 4, 128).transpose(2, 1, 0).reshape(128, 12)).astype(np.float32)
    c["c_qkg"] = np.stack([np.tile(inputs["od_q_gain"][0], 2), np.tile(inputs["od_k_gain"][0], 2)], axis=1).astype(np.float32)
    rb = inputs["rel_bias"]
    c["c_c31"] = np.ascontiguousarray(np.broadcast_to(rb[31][None, :], (128, 16))).astype(np.float32)
    c["c_eps"] = np.ascontiguousarray(np.broadcast_to(np.array([EPS, 64 * EPS], np.float32)[None, :], (128, 2)))
    c["c_one"] = np.ones((128, 1), np.float32)
    c["c_ident"] = np.eye(128, dtype=np.float32).astype(bf)
    c["c_ustrict"] = (s > t).astype(np.float32).astype(bf)
    c["c_ones"] = np.ones((128, 128), np.float32).astype(bf)
    blk = np.zeros((128, 128), np.float32)
    blk[:64, :64] = 1
    blk[64:, 64:] = 1
    c["c_blk"] = blk.astype(bf)
    c["c_ncT"] = (s > t).astype(np.float32).astype(bf)
    c["c_negI"] = (-BIG * np.eye(128, dtype=np.float32)).astype(bf)
    u = np.arange(256)[None, :]
    dist = np.maximum(u - s, 0)
    bidx = _rel_bucket(dist)
    tab = rb[bidx]
    c["biasT"] = np.ascontiguousarray(tab.transpose(0, 2, 1).reshape(128, 16 * 256)).astype(np.float32)
    return c


_CACHE = {}


def run_parts(parts, xT_list, inputs, consts):
    key = tuple(parts)
    if key not in _CACHE:
        _CACHE[key] = build(parts)
    nc, K = _CACHE[key]
    base = {k: np.ascontiguousarray(np.asarray(inputs[k])[0] if k in ("ev_w_in", "ev_w_out", "od_w_in", "od_w_out") else np.asarray(inputs[k]))
            for k in ("ev_w_in", "ev_w_out", "od_w_in", "od_w_out", "ffn_w_gate", "ffn_w_up", "ffn_w_down")}
    base.update(consts)
    in_maps = []
    for xT in xT_list:
        m = dict(base)
        m["xT"] = xT
        in_maps.append(m)
    res = run_bass_kernel_spmd(nc, in_maps, core_ids=list(range(len(xT_list))))
    return [r["yT"] for r in res.results]


PLAN = [("l0mix", "l0ffn", "l1mix", "l1ffn")]


def kernel(**inputs):
    inputs = {k: np.asarray(v) for k, v in inputs.items()}
    x = inputs["x"]
    consts = host_consts(inputs)
    xTs = [np.ascontiguousarray(x[b].T) for b in range(x.shape[0])]
    for parts in PLAN:
        xTs = run_parts(parts, xTs, inputs, consts)
    out = np.stack([np.ascontiguousarray(y.T) for y in xTs], axis=0)
    return out.astype(np.float32)
```
